# Optimizing a Trainium2 kernel written in Bass

```python
import jax, jax.numpy as jnp
from jax import lax
import numpy as np

D_MODEL = 1024
BATCH = 2
SEQ = 8192
DEPTH = 1

N_HEADS = 8
HEAD_DIM = 64
N_KV = 2
D_ATTN = N_HEADS * HEAD_DIM
D_KV = N_KV * HEAD_DIM
N_GM_GROUPS = 8
GM_GROUP_DIM = 64
D_GM = N_GM_GROUPS * GM_GROUP_DIM
D_MIX = D_ATTN + D_GM
SPLIT_SIZES = (D_ATTN, D_KV, D_KV, D_KV, D_KV, D_KV, D_KV, 3 * N_HEADS, D_GM, D_GM)
D_IN = D_ATTN + 6 * D_KV + 3 * N_HEADS + 2 * D_GM
CMP_LEN = 32
CMP_STRIDE = 16
CMP_HIDDEN = 128
SEL_BLOCK = 64
N_SEL = 16
WINDOW = 512
Q_BLOCK = 128
FORCE_BONUS = 1.0e4
GM_CHUNK = 128
N_EXPERTS = 32
TOP_K = 4
D_EXPERT = 1024
SWIGLU_LIMIT = 7.0
SWIGLU_ALPHA = 1.702
MOE_BLOCK = 128

EPS = 1e-6
NEG = -1.0e30

kernel_name = "hymba_nsa_gmlp_moe_layer"


def rmsnorm(x, g):
    xf = x.astype(jnp.float32)
    r = lax.rsqrt(jnp.mean(xf * xf, axis=-1, keepdims=True) + EPS)
    return (xf * r).astype(x.dtype) * g


def masked_softmax(s, mask):
    s = jnp.where(mask, s.astype(jnp.float32), NEG)
    p = jax.nn.softmax(s, axis=-1)
    return jnp.where(mask, p, 0.0)


def alibi_slopes(n_heads):
    return jnp.exp2(-8.0 * jnp.arange(1, n_heads + 1, dtype=jnp.float32) / n_heads)


def compress(kv, pos_emb, w1, b1, w2, b2):
    B, G, T, DH = kv.shape
    seg = kv.reshape(B, G, T // CMP_STRIDE, CMP_STRIDE, DH)
    r = CMP_LEN // CMP_STRIDE
    nc = T // CMP_STRIDE - r + 1
    blocks = jnp.concatenate([seg[:, :, i:i + nc] for i in range(r)], axis=3)
    flat = (blocks + pos_emb).reshape(B, G, nc, CMP_LEN * DH)
    hid = jax.nn.gelu(flat @ w1 + b1)
    return hid @ w2 + b2


def block_overlap(nc, ns):
    c0 = jnp.arange(nc)[:, None] * CMP_STRIDE
    n0 = jnp.arange(ns)[None, :] * SEL_BLOCK
    ov = jnp.clip(jnp.minimum(c0 + CMP_LEN, n0 + SEL_BLOCK) - jnp.maximum(c0, n0), 0, None)
    return ov.astype(jnp.float32) / CMP_LEN


def nsa_attention(q, kc, vc, ks, vs, kw, vw, gates):
    B, H, T, DH = q.shape
    G = ks.shape[1]
    R = H // G
    NC = kc.shape[2]
    NS = T // SEL_BLOCK
    n_sel = min(N_SEL, NS)
    scale = DH ** -0.5
    slopes = alibi_slopes(H).reshape(G, R, 1, 1)
    c_end = jnp.arange(NC) * CMP_STRIDE + CMP_LEN - 1
    overlap = block_overlap(NC, NS)
    n_start = jnp.arange(NS) * SEL_BLOCK
    ks_blocks = ks.reshape(B, G, NS, SEL_BLOCK, DH)
    vs_blocks = vs.reshape(B, G, NS, SEL_BLOCK, DH)
    pad = ((0, 0), (0, 0), (WINDOW, 0), (0, 0))
    kw_pad = jnp.pad(kw, pad)
    vw_pad = jnp.pad(vw, pad)
    b_ix = jnp.arange(B)[:, None, None, None]
    g_ix = jnp.arange(G)[None, :, None, None]
    in_block = jnp.arange(SEL_BLOCK)
    win_off = jnp.arange(WINDOW + Q_BLOCK) - WINDOW

    def one_block(q0):
        t = q0 + jnp.arange(Q_BLOCK)
        qg = lax.dynamic_slice_in_dim(q, q0, Q_BLOCK, axis=2).reshape(B, G, R, Q_BLOCK, DH)
        gt = lax.dynamic_slice_in_dim(gates, q0, Q_BLOCK, axis=2).reshape(B, G, R, Q_BLOCK, 3)
        dist_c = t[:, None] - c_end[None, :]
        s = jnp.einsum('bgrqd,bgcd->bgrqc', qg, kc) * scale - slopes * dist_c.astype(jnp.float32)
        p_cmp = masked_softmax(s, dist_c >= 0)
        o_cmp = jnp.einsum('bgrqc,bgcd->bgrqd', p_cmp.astype(vc.dtype), vc)
        imp = jnp.einsum('bgrqc,cn->bgqn', p_cmp, overlap)
        forced = (n_start[None, :] == (t[:, None] // SEL_BLOCK) * SEL_BLOCK) | (n_start[None, :] == 0)
        imp = jnp.where(forced, imp + FORCE_BONUS, imp)
        imp = jnp.where(n_start[None, :] <= t[:, None], imp, NEG)
        _, idx = lax.top_k(imp, n_sel)
        k_g = ks_blocks[b_ix, g_ix, idx].reshape(B, G, Q_BLOCK, n_sel * SEL_BLOCK, DH)
        v_g = vs_blocks[b_ix, g_ix, idx].reshape(B, G, Q_BLOCK, n_sel * SEL_BLOCK, DH)
        pos = (idx[..., None] * SEL_BLOCK + in_block).reshape(B, G, Q_BLOCK, n_sel * SEL_BLOCK)
        dist_s = (t[None, None, :, None] - pos)[:, :, None]
        s = jnp.einsum('bgrqd,bgqkd->bgrqk', qg, k_g) * scale - slopes * dist_s.astype(jnp.float32)
        p = masked_softmax(s, dist_s >= 0)
        o_sel = jnp.einsum('bgrqk,bgqkd->bgrqd', p.astype(v_g.dtype), v_g)
        k_w = lax.dynamic_slice_in_dim(kw_pad, q0, WINDOW + Q_BLOCK, axis=2)
        v_w = lax.dynamic_slice_in_dim(vw_pad, q0, WINDOW + Q_BLOCK, axis=2)
        pos_w = q0 + win_off
        dist_w = t[:, None] - pos_w[None, :]
        mask_w = (dist_w >= 0) & (dist_w < WINDOW) & (pos_w[None, :] >= 0)
        s = jnp.einsum('bgrqd,bgkd->bgrqk', qg, k_w) * scale - slopes * dist_w.astype(jnp.float32)
        p = masked_softmax(s, mask_w)
        o_win = jnp.einsum('bgrqk,bgkd->bgrqd', p.astype(v_w.dtype), v_w)
        o = gt[..., 0:1] * o_cmp + gt[..., 1:2] * o_sel + gt[..., 2:3] * o_win
        return o.reshape(B, H, Q_BLOCK, DH)

    out = lax.map(one_block, jnp.arange(T // Q_BLOCK) * Q_BLOCK)
    return out.transpose(1, 0, 3, 2, 4).reshape(B, T, H * DH)


def spatial_gating(u, v, v_gain, w_s, b_s):
    B, T, _ = u.shape
    u = jax.nn.gelu(u)
    v = rmsnorm(jax.nn.gelu(v), v_gain)
    vc = v.reshape(B, T // GM_CHUNK, GM_CHUNK, N_GM_GROUPS, GM_GROUP_DIM)
    ws = w_s * jnp.tril(jnp.ones((GM_CHUNK, GM_CHUNK), w_s.dtype))
    y = jnp.einsum('gts,bcsgd->bctgd', ws, vc) + b_s.T[None, None, :, :, None]
    return u * y.reshape(B, T, D_GM)


def moe(x, w_router, b_router, w_gate_up, b_gate_up, w_down, b_down):
    B, T, D = x.shape
    N = B * T
    xt = x.reshape(N, D)
    logits = (xt @ w_router + b_router).astype(jnp.float32)
    top_val, top_idx = lax.top_k(logits, TOP_K)
    gate = jax.nn.softmax(top_val, axis=-1)
    S = N * TOP_K
    e_flat = top_idx.reshape(S)
    tok_flat = jnp.repeat(jnp.arange(N, dtype=jnp.int32), TOP_K)
    g_flat = gate.reshape(S)
    order = jnp.argsort(e_flat)
    e_s, tok_s, g_s = e_flat[order], tok_flat[order], g_flat[order]
    counts = jnp.bincount(e_flat, length=N_EXPERTS)
    padded = ((counts + MOE_BLOCK - 1) // MOE_BLOCK) * MOE_BLOCK
    pad_end = jnp.cumsum(padded)
    pad_start = pad_end - padded
    cnt_start = jnp.cumsum(counts) - counts
    dest = pad_start[e_s] + jnp.arange(S) - cnt_start[e_s]
    P = ((S + MOE_BLOCK - 1) // MOE_BLOCK) * MOE_BLOCK + N_EXPERTS * MOE_BLOCK
    NB = P // MOE_BLOCK
    tok_buf = jnp.full((P,), N, dtype=jnp.int32).at[dest].set(tok_s)
    g_buf = jnp.zeros((P,), jnp.float32).at[dest].set(g_s)
    blk_expert = jnp.minimum(jnp.searchsorted(pad_end, jnp.arange(NB) * MOE_BLOCK, side='right'), N_EXPERTS - 1)
    x_pad = jnp.concatenate([xt, jnp.zeros((1, D), xt.dtype)], axis=0)

    def expert_block(args):
        e, tok, g = args
        xb = x_pad[tok]
        h = xb @ w_gate_up[e] + b_gate_up[e]
        h_glu = jnp.minimum(h[:, 0::2], SWIGLU_LIMIT)
        h_lin = jnp.clip(h[:, 1::2], -SWIGLU_LIMIT, SWIGLU_LIMIT)
        a = h_glu * jax.nn.sigmoid(SWIGLU_ALPHA * h_glu) * (h_lin + 1.0)
        return ((a @ w_down[e] + b_down[e]) * g[:, None]).astype(x.dtype)

    out = lax.map(expert_block, (blk_expert, tok_buf.reshape(NB, MOE_BLOCK), g_buf.reshape(NB, MOE_BLOCK)))
    y = jnp.zeros((N + 1, D), x.dtype).at[tok_buf].add(out.reshape(P, D))
    return y[:N].reshape(B, T, D)


def setup_inputs(seed: int = 0) -> dict:
    key = jax.random.key(seed)
    ks = jax.random.split(key, 23)
    f = jnp.float32

    def nrm(k, shape, scale):
        return jax.random.normal(k, shape, f) * scale

    def gain(k, shape):
        return 1.0 + 0.01 * jax.random.normal(k, shape, f)

    return {
        "x": nrm(ks[0], (BATCH, SEQ, D_MODEL), 1.0),
        "norm1_g": gain(ks[1], (D_MODEL,)),
        "w_in": nrm(ks[2], (D_MODEL, D_IN), D_MODEL ** -0.5),
        "q_norm_g": gain(ks[3], (HEAD_DIM,)),
        "k_norm_g": gain(ks[4], (3, HEAD_DIM)),
        "cmp_pos": nrm(ks[5], (2, CMP_LEN, HEAD_DIM), 0.02),
        "w_cmp1": nrm(ks[6], (2, CMP_LEN * HEAD_DIM, CMP_HIDDEN), (CMP_LEN * HEAD_DIM) ** -0.5),
        "b_cmp1": nrm(ks[7], (2, CMP_HIDDEN), 0.01),
        "w_cmp2": nrm(ks[8], (2, CMP_HIDDEN, HEAD_DIM), CMP_HIDDEN ** -0.5),
        "b_cmp2": nrm(ks[9], (2, HEAD_DIM), 0.01),
        "gm_v_norm_g": gain(ks[10], (D_GM,)),
        "gm_w_s": nrm(ks[11], (N_GM_GROUPS, GM_CHUNK, GM_CHUNK), GM_CHUNK ** -0.5),
        "gm_b_s": gain(ks[12], (N_GM_GROUPS, GM_CHUNK)),
        "out_norm_attn_g": gain(ks[13], (D_ATTN,)),
        "out_norm_gm_g": gain(ks[14], (D_GM,)),
        "w_out": nrm(ks[15], (D_MIX, D_MODEL), D_MIX ** -0.5),
        "norm2_g": gain(ks[16], (D_MODEL,)),
        "w_router": nrm(ks[17], (D_MODEL, N_EXPERTS), D_MODEL ** -0.5),
        "b_router": nrm(ks[18], (N_EXPERTS,), 0.01),
        "w_gate_up": nrm(ks[19], (N_EXPERTS, D_MODEL, 2 * D_EXPERT), D_MODEL ** -0.5),
        "b_gate_up": nrm(ks[20], (N_EXPERTS, 2 * D_EXPERT), 0.01),
        "w_down": nrm(ks[21], (N_EXPERTS, D_EXPERT, D_MODEL), D_EXPERT ** -0.5),
        "b_down": nrm(ks[22], (N_EXPERTS, D_MODEL), 0.01),
    }


def reference(x, norm1_g, w_in, q_norm_g, k_norm_g, cmp_pos, w_cmp1, b_cmp1, w_cmp2, b_cmp2,
              gm_v_norm_g, gm_w_s, gm_b_s, out_norm_attn_g, out_norm_gm_g, w_out, norm2_g,
              w_router, b_router, w_gate_up, b_gate_up, w_down, b_down):
    B, T, _ = x.shape

    def heads(a, n):
        return a.reshape(B, T, n, HEAD_DIM).transpose(0, 2, 1, 3)

    for _layer in range(DEPTH):
        h = rmsnorm(x, norm1_g)
        z = h @ w_in
        split_at = np.cumsum(SPLIT_SIZES)[:-1].tolist()
        zq, zkc, zvc, zks, zvs, zkw, zvw, zg, zu, zv = jnp.split(z, split_at, axis=-1)
        q = rmsnorm(heads(zq, N_HEADS), q_norm_g)
        kc = rmsnorm(compress(heads(zkc, N_KV), cmp_pos[0], w_cmp1[0], b_cmp1[0], w_cmp2[0], b_cmp2[0]), k_norm_g[0])
        vc = compress(heads(zvc, N_KV), cmp_pos[1], w_cmp1[1], b_cmp1[1], w_cmp2[1], b_cmp2[1])
        k_sel = rmsnorm(heads(zks, N_KV), k_norm_g[1])
        k_win = rmsnorm(heads(zkw, N_KV), k_norm_g[2])
        gates = jax.nn.sigmoid(zg).reshape(B, T, N_HEADS, 3).transpose(0, 2, 1, 3)
        o_attn = nsa_attention(q, kc, vc, k_sel, heads(zvs, N_KV), k_win, heads(zvw, N_KV), gates)
        o_gm = spatial_gating(zu, zv, gm_v_norm_g, gm_w_s, gm_b_s)
        mixed = jnp.concatenate([rmsnorm(o_attn, out_norm_attn_g), rmsnorm(o_gm, out_norm_gm_g)], axis=-1)
        x = x + mixed @ w_out
        x = x + moe(rmsnorm(x, norm2_g), w_router, b_router, w_gate_up, b_gate_up, w_down, b_down)
    return x
```

```python
import numpy as np
from contextlib import ExitStack
import concourse.bass as bass
import concourse.mybir as mybir
from concourse.bass_utils import run_bass_kernel_spmd

F32 = mybir.dt.float32
BF16 = mybir.dt.bfloat16
AF = mybir.ActivationFunctionType
ALU = mybir.AluOpType
AX = mybir.AxisListType

ENG = ['pe', 'act', 'dve', 'pool', 'sp']
BLK = {'pe': 'tensor', 'act': 'scalar', 'dve': 'vector', 'pool': 'gpsimd', 'sp': 'sync'}


class Sched:
    def __init__(self, nc, stack):
        self.nc = nc
        self.stack = stack
        self.sem = {e: stack.enter_context(nc.semaphore("c_" + e)) for e in ENG}
        self.cnt = {e: 0 for e in ENG}
        self.prog = {e: [] for e in ENG}
        self.waited = {e: {} for e in ENG}
        self.lastw = {}
        self.readers = {}
        self.dsem = {}
        self.dcnt = {}
        self.dname = {}
        self.nops = {e: 0 for e in ENG}

    def _wait(self, eng, tok):
        sem, val, kind = tok
        if kind == 'dma':
            val = self.dcnt[self.dname[sem.num]]
        w = self.waited[eng]
        if w.get(sem.num, 0) >= val:
            return
        w[sem.num] = val
        self.prog[eng].append(lambda e, s=sem, v=val: e.wait_ge(s, v))

    def _deps(self, eng, reads, writes):
        for k in reads:
            t = self.lastw.get(k)
            if t is not None:
                self._wait(eng, t)
        for k in writes:
            t = self.lastw.get(k)
            if t is not None and (t[2] != eng or eng != 'pe'):
                self._wait(eng, t)
            for t in self.readers.get(k, {}).values():
                if t[2] != eng or eng != 'pe':
                    self._wait(eng, t)

    def _commit(self, tok, reads, writes):
        for k in reads:
            self.readers.setdefault(k, {})[tok[0].num] = tok
        for k in writes:
            self.lastw[k] = tok
            self.readers[k] = {}

    def op(self, eng, fn, reads=(), writes=(), signal=True):
        self._deps(eng, reads, writes)
        sem = self.sem[eng]
        self.nops[eng] += 1
        if signal:
            self.cnt[eng] += 1
            tok = (sem, self.cnt[eng], eng)
            self.prog[eng].append(lambda e, f=fn, s=sem: f(e).then_inc(s, 1))
        else:
            tok = (sem, self.cnt[eng] + 1, eng)
            self.prog[eng].append(lambda e, f=fn: f(e))
        self._commit(tok, reads, writes)

    def dma(self, q, out, in_, reads=(), writes=(), name=None, **kw):
        self._deps(q, reads, writes)
        if name not in self.dsem:
            self.dsem[name] = self.stack.enter_context(self.nc.semaphore("d_" + str(name)))
            self.dcnt[name] = 0
            self.dname[self.dsem[name].num] = name
        sem = self.dsem[name]
        self.dcnt[name] += 16
        tok = (sem, self.dcnt[name], 'dma')
        self.prog[q].append(
            lambda e, o=out, i=in_, s=sem, kw=kw: e.dma_start(out=o, in_=i, **kw).then_inc(s, 16))
        self._commit(tok, reads, writes)
        return tok

    def barrier(self):
        toks = [(self.sem[e], self.cnt[e], e) for e in ENG if self.cnt[e] > 0]
        toks += [(self.dsem[n], self.dcnt[n], 'dma') for n in self.dsem]
        for e in ENG:
            for t in toks:
                self._wait(e, t)

    def finish(self, eng='sp'):
        for n in self.dsem:
            self._wait(eng, (self.dsem[n], self.dcnt[n], 'dma'))
        for e in ENG:
            if e != eng and self.cnt[e] > 0:
                self._wait(eng, (self.sem[e], self.cnt[e], e))

    def emit(self):
        nc = self.nc
        with nc.Block() as block:
            for e in ENG:
                prog = self.prog[e]
                if not prog:
                    continue

                def body(engine, prog=prog):
                    for f in prog:
                        f(engine)
                getattr(block, BLK[e])(body)


class Arena:
    def __init__(self, nc, nbytes):
        self.t = nc.alloc_sbuf_tensor("arena", [128, nbytes // 2], BF16)
        self.top = 0
        self.cap = nbytes
        self.peak = 0

    def alloc(self, shape_free, dtype):
        n = int(np.prod(shape_free))
        esz = 4 if dtype == F32 else 2
        nb = (n * esz + 31) // 32 * 32
        off = self.top
        self.top += nb
        self.peak = max(self.peak, self.top)
        assert self.top <= self.cap, f"SBUF arena overflow {self.top} > {self.cap}"
        ap = self.t[:, off // 2: off // 2 + n * esz // 2]
        if dtype == F32:
            ap = ap.bitcast(F32)
        if len(shape_free) > 1:
            names = " ".join(f"d{i}" for i in range(len(shape_free)))
            kw = {f"d{i}": int(s) for i, s in enumerate(shape_free)}
            ap = ap.rearrange(f"p ({names}) -> p {names}", **kw)
        return ap

    def view(self, off, shape_free, dtype):
        n = int(np.prod(shape_free))
        esz = 4 if dtype == F32 else 2
        ap = self.t[:, off // 2: off // 2 + n * esz // 2]
        if dtype == F32:
            ap = ap.bitcast(F32)
        if len(shape_free) > 1:
            names = " ".join(f"d{i}" for i in range(len(shape_free)))
            kw = {f"d{i}": int(s) for i, s in enumerate(shape_free)}
            ap = ap.rearrange(f"p ({names}) -> p {names}", **kw)
        return ap

    def mark(self):
        return self.top

    def release(self, m):
        self.top = m


D = 1024
NH = 8
DH = 64
NSLOT = 64
NOWN = 16
NE = 32
EPS = 1e-6
NEG = -1.0e30
N_EXP_RUN = 32
DEBUG = False
import os as _os
A_LEVEL = int(_os.environ.get('A_LEVEL', '9'))
D_LEVEL = int(_os.environ.get('D_LEVEL', '9'))

_PERM = np.concatenate([np.arange(768, 896), np.arange(1024, 1152), np.arange(512, 640), np.arange(640, 768),
                        np.arange(896, 1024), np.arange(1152, 1280), np.arange(0, 512), np.arange(1280, 1304),
                        np.arange(1304, 1816), np.arange(1816, 2328)])
NROWA = 64 + 192 + 512 + 512 + 512 + 128
NROWD = 1024 + 32


def _host_consts(j):
    pad = 3 - j
    f = np.float32
    p = np.arange(128)
    slopes = (2.0 ** (-(np.arange(8) + 1.0))).astype(np.float64)
    dt = np.arange(64)
    biasT = slopes[None, None, :] * (-128.0 * dt[None, :, None] + p[:, None, None] - 127.0)
    ii = np.arange(16)
    ct = np.arange(4)
    cend = 16.0 * (128 * ct[None, None, :, None] + p[:, None, None, None]) + 31.0
    tref = 128.0 * (4 * ii[None, :, None, None] + 3) + 127.0
    biasC = np.minimum(slopes[None, None, None, :] * (cend - tref), 0.0)
    q = np.arange(128)
    cendd = 16 * (128 * (ii[None, :, None] // 4) + p[:, None, None]) + 31
    tq = 128 * (4 * ii[None, :, None] + 3) + q[None, None, :]
    cmaskT = (cendd <= tq).astype(f)
    cidx = 128 * ct[None, :] + p[:, None]
    cvalid = ((cidx >= 8 * pad) & (cidx <= 510)).astype(f)
    n = np.arange(128)
    c0 = 16 * cidx[:, :, None]
    n0 = 64 * n[None, None, :]
    ov = np.clip(np.minimum(c0 + 32, n0 + 64) - np.maximum(c0, n0), 0, None).astype(f) / 32.0
    ovaug = np.concatenate([cvalid[:, :, None], ov * cvalid[:, :, None]], axis=2)
    t = 128 * (4 * ii[:, None, None] + 3) + q[None, :, None]
    nn = n[None, None, :]
    forced = (nn == t // 64) | (nn == 2 * pad)
    invalid = (64 * nn > t) | (nn < 2 * pad)
    addmask = np.where(invalid, NEG, np.where(forced, 1.0e4, 0.0)).astype(f)
    addmask = np.ascontiguousarray(addmask.transpose(1, 0, 2))
    kvalid = np.broadcast_to((np.arange(64) >= pad).astype(f)[None, :], (128, 64)).copy()
    tri = (p[:, None] <= q[None, :]).astype(f)
    triu = (p[:, None] > q[None, :]).astype(f)
    return dict(biasT=biasT.astype(f).reshape(128, 512), biasC=biasC.astype(f).reshape(128, 512),
                cmaskT=cmaskT.reshape(128, 2048), cvalid=cvalid, ovaug=ovaug.reshape(128, 4 * 129),
                addmask=addmask.reshape(128, 2048), kvalid=kvalid,
                trim=np.concatenate([tri, triu], axis=1))


def _host_weights(inp):
    f = np.float32
    w = {}
    w_in = inp["w_in"][:, _PERM]
    w["w_in"] = np.ascontiguousarray(w_in.reshape(8, 128, 2328).transpose(1, 0, 2))
    w["g1"] = np.ascontiguousarray(inp["norm1_g"].reshape(8, 128).T)
    kg = inp["k_norm_g"]
    w["rowA"] = np.concatenate([inp["q_norm_g"], kg[1], kg[2], kg[0], inp["gm_v_norm_g"], inp["out_norm_attn_g"],
                                inp["out_norm_gm_g"], inp["b_cmp2"][0], inp["b_cmp2"][1]]).astype(f)[None, :]
    w["rowD"] = np.concatenate([inp["norm2_g"], inp["b_router"]]).astype(f)[None, :]
    w1 = inp["w_cmp1"].reshape(2, 32, 64, 128).transpose(2, 0, 1, 3)
    w["w1s"] = np.ascontiguousarray(np.concatenate([w1, w1], axis=0)).reshape(128, 2 * 32 * 128)
    pos = inp["cmp_pos"].transpose(2, 0, 1)
    w["posT"] = np.ascontiguousarray(np.concatenate([pos, pos], axis=0)).reshape(128, 64)
    w["b1c"] = np.ascontiguousarray(inp["b_cmp1"].T)
    w["w2"] = np.ascontiguousarray(inp["w_cmp2"].transpose(1, 0, 2)).reshape(128, 128)
    w["wsT"] = np.ascontiguousarray(inp["gm_w_s"].transpose(2, 0, 1)).reshape(128, 1024)
    w["bsT"] = np.ascontiguousarray(inp["gm_b_s"].T)
    w["w_out"] = np.ascontiguousarray(inp["w_out"].reshape(8, 128, 1024).transpose(1, 0, 2)).reshape(128, 8192)
    w["w_router"] = np.ascontiguousarray(inp["w_router"].reshape(8, 128, 32).transpose(1, 0, 2)).reshape(128, 256)
    wgu = inp["w_gate_up"].reshape(NE, 8, 128, 8, 128, 2).transpose(0, 3, 2, 1, 5, 4)
    w["wgu"] = np.ascontiguousarray(wgu).reshape(NE * 8, 128, 8 * 2 * 128)
    w["bgu"] = np.ascontiguousarray(inp["b_gate_up"].reshape(NE, 8, 128, 2).transpose(2, 0, 1, 3)).reshape(128, NE * 16)
    w["wd"] = np.ascontiguousarray(inp["w_down"].reshape(NE, 8, 128, 1024).transpose(0, 2, 1, 3)).reshape(NE, 128, 8192)
    w["b_down"] = np.ascontiguousarray(inp["b_down"])
    return w


def build_program(n_exp=N_EXP_RUN, debug=DEBUG, stop_after=None):
    nc = bass.Bass("TRN2", target_bir_lowering=False)

    def din(name, shape):
        return nc.dram_tensor(name, shape, F32, kind="ExternalInput").ap()

    xT_d = din("xT", [64, 128, 1024])
    xown_d = din("xown", [16, 128, 1024])
    biasT_d = din("biasT", [128, 512])
    biasC_d = din("biasC", [128, 512])
    cmaskT_d = din("cmaskT", [128, 2048])
    cvalid_d = din("cvalid", [128, 4])
    ovaug_d = din("ovaug", [128, 516])
    addmask_d = din("addmask", [128, 2048])
    kvalid_d = din("kvalid", [128, 64])
    trim_d = din("trim", [128, 256])
    w_in_d = din("w_in", [128, 8, 2328])
    g1_d = din("g1", [128, 8])
    rowA_d = din("rowA", [1, NROWA])
    rowD_d = din("rowD", [1, NROWD])
    w1s_d = din("w1s", [128, 8192])
    posT_d = din("posT", [128, 64])
    b1c_d = din("b1c", [128, 2])
    w2_d = din("w2", [128, 128])
    wsT_d = din("wsT", [128, 1024])
    bsT_d = din("bsT", [128, 8])
    w_out_d = din("w_out", [128, 8192])
    w_router_d = din("w_router", [128, 256])
    wgu_d = din("wgu", [max(n_exp, 1) * 8, 128, 2048])
    bgu_d = din("bgu", [128, NE * 16])
    wd_d = din("wd", [max(n_exp, 1), 128, 8192])
    b_down_d = din("b_down", [NE, 1024])
    out_d = nc.dram_tensor("out", [16, 128, 1024], F32, kind="ExternalOutput").ap()
    dbg_outs = {}

    st = ExitStack()
    with st:
        S = Sched(nc, st)
        A = Arena(nc, 191 * 1024)
        ps = [nc.alloc_psum_tensor(f"ps{b}", [128, 512], F32) for b in range(8)]
        P = [p_[:, :] for p_ in ps]
        PB = [p_[:, :].bitcast(BF16) for p_ in ps]

        def _finish():
            S.finish('sp')
            S.emit()
            print("SBUF peak", A.peak, "ops", S.nops, "cnt", S.cnt)
            return nc, dbg_outs

        def dbg(name, ap, n, reads=()):
            if not debug:
                return
            t = nc.dram_tensor("dbg_" + name, [128, n], F32, kind="ExternalOutput").ap()
            dbg_outs[name] = t
            tmp = A.alloc([n], F32)
            S.op('dve', lambda e, o=tmp, i=ap: e.tensor_copy(out=o, in_=i), reads=list(reads), writes=['dbg_' + name])
            S.dma('sp', t, tmp, reads=['dbg_' + name], name='out')
            S.barrier()

        eps_holder = [None]

        def rsqrt_act(out_ap, in_ap, scale, reads, writes):
            S.op('act', lambda e, o=out_ap, i=in_ap, s=scale, b_=eps_holder[0]: e.activation(out=o, in_=i, func=AF.Ln, bias=b_, scale=s),
                 reads=reads, writes=writes)
            S.op('act', lambda e, o=out_ap: e.activation(out=o, in_=o, func=AF.Exp, scale=-0.5),
                 reads=writes, writes=writes)

        identb = A.alloc([128], BF16)
        identf = A.alloc([128], F32)
        onesb = A.alloc([8], BF16)
        epsc = A.alloc([8], F32)
        EPS_AP = epsc[:, 0:1]
        eps_holder[0] = EPS_AP
        rowA = A.alloc([NROWA], F32)
        qg8 = A.alloc([64], F32)
        biasT = A.alloc([64, 8], F32)
        biasC = A.alloc([16, 4, 8], F32)
        cmaskT = A.alloc([16, 128], BF16)
        trim = A.alloc([2, 128], BF16)
        cvalid = A.alloc([4], F32)
        rown = A.alloc([16], F32)
        Kst = A.alloc([2, 8192], BF16)
        Vaug = A.alloc([2, 64, 2, 65], BF16)
        KcT = A.alloc([512], BF16)
        Rc = A.alloc([4, 2, 193], BF16)
        kvt = A.alloc([64], F32)
        base_top = A.mark()

        S.op('pool', lambda e: e.memset(identf, 1.0), writes=['identf'])
        S.op('pool', lambda e: e.affine_select(out=identf, in_=identf, pattern=[[-1, 128]], compare_op=ALU.is_equal,
                                              fill=0.0, base=0, channel_multiplier=1), reads=['identf'], writes=['identf'])
        S.op('dve', lambda e: e.tensor_copy(out=identb, in_=identf), reads=['identf'], writes=['identb'])
        S.op('dve', lambda e: e.memset(onesb, 1.0), writes=['onesb'])
        S.op('dve', lambda e: e.memset(epsc, EPS), writes=['epsc'])
        S.dma('sp', rowA, rowA_d[0:1, :].to_broadcast([128, NROWA]), writes=['c0'], name='c0')
        S.dma('sp', biasT.rearrange("p a b -> p (a b)"), biasT_d, writes=['c0'], name='c0')
        S.dma('sp', biasC.rearrange("p a b c -> p (a b c)"), biasC_d, writes=['c0'], name='c0')
        S.dma('sp', cvalid, cvalid_d, writes=['c0'], name='c0')
        S.dma('pool', cmaskT.rearrange("p a b -> p (a b)"), cmaskT_d, writes=['c0p'], name='c0p')
        S.dma('pool', trim.rearrange("p a b -> p (a b)"), trim_d, writes=['c0p'], name='c0p')
        S.dma('sp', kvt, kvalid_d, writes=['kvt'], name='c0b')
        for kind in range(2):
            for g in range(2):
                S.op('dve', lambda e, kind=kind, g=g: e.tensor_copy(out=Vaug[:, kind, :, g, 64:65], in_=kvt.unsqueeze(2)),
                     reads=['kvt'], writes=['Vcol'])
        ov_v = ovaug_d.rearrange("p (c n) -> p c n", n=129)
        for g in range(2):
            S.dma('pool', Rc[:, :, g, 64:193], ov_v, writes=['c0p'], name='c0p')
        S.op('dve', lambda e: e.tensor_scalar(out=qg8, in0=rowA[:, 0:64], scalar1=0.125, scalar2=None, op0=ALU.mult),
             reads=['c0'], writes=['qg8'])
        if stop_after == 'init':
            S.barrier()
            dbg("rowA", rowA[:, 0:256], 256)
            dbg("Rc0", Rc[:, 0, :, :].rearrange("p g d -> p (g d)"), 386)
            dbg("cmask", cmaskT[:, 3, :], 128)
            dbg("Vcol", Vaug[:, 0, :, 0, 64:65].rearrange("p a b -> p (a b)"), 64)
            return _finish()
        kg12 = rowA[:, 64:192].rearrange("p (k d) -> p k d", d=64)
        kg0 = rowA[:, 192:256]
        gmv = rowA[:, 256:768]
        ona = rowA[:, 768:1280]
        ong = rowA[:, 1280:1792]
        b2k = rowA[:, 1792:1856]
        b2v = rowA[:, 1856:1920]

        KCraw = A.alloc([2, 8224], BF16)
        Wkv = A.alloc([8, 768], BF16)
        g1c = A.alloc([8], F32)
        w1s = A.alloc([2, 32, 128], BF16)
        posTb = A.alloc([2, 32], BF16)
        b1c = A.alloc([2], F32)
        w2b = A.alloc([2, 64], BF16)
        xTb = [A.alloc([8, 128], BF16) for _ in range(2)]
        sqb = [A.alloc([8, 128], BF16) for _ in range(2)]
        z1 = [A.alloc([256], F32) for _ in range(2)]
        zt = [A.alloc([256], F32) for _ in range(2)]
        ktok = [A.alloc([512], BF16) for _ in range(2)]
        rr = [A.alloc([8], F32) for _ in range(2)]
        Wkv32 = [A.alloc([768], F32) for _ in range(2)]

        S.dma('sp', g1c, g1_d, writes=['g1c'], name='c1')
        S.dma('sp', b1c, b1c_d, writes=['b1c'], name='c1')
        S.dma('pool', w1s.rearrange("p a b c -> p (a b c)"), w1s_d, writes=['w1s'], name='c1p')
        S.dma('pool', posTb.rearrange("p a b -> p (a b)"), posT_d, writes=['posTb'], name='c1p')
        S.dma('pool', w2b.rearrange("p a b -> p (a b)"), w2_d, writes=['w2b'], name='c1p')
        for kc in range(8):
            wb = Wkv32[kc % 2]
            S.dma('sp', wb, w_in_d[:, kc, 0:768], writes=[f'Wkv32{kc%2}'], name=f'Wkv32{kc%2}')
            S.op('dve', lambda e, kc=kc, wb=wb: e.tensor_scalar(out=Wkv[:, kc, :], in0=wb, scalar1=g1c[:, kc:kc + 1],
                                                                scalar2=None, op0=ALU.mult),
                 reads=[f'Wkv32{kc%2}', 'g1c'], writes=['Wkv'])
        S.op('pool', lambda e: e.memset(KCraw[:, :, 8192:8224], 0.0), writes=['KCpad'])
        if NSLOT < 64:
            S.op('pool', lambda e: e.memset(KCraw[:, :, NSLOT * 128:8192], 0.0), writes=['KCpad'])

        for s in range(NSLOT):
            b = s % 2
            xb, sq, z1b, ztb, kt_, rb = xTb[b], sqb[b], z1[b], zt[b], ktok[b], rr[b]
            pA, pB, pT = P[3 * b], P[3 * b + 1], PB[3 * b + 2]
            kx, ksq, kz, kzt, kk, kr = f'xTb{b}', f'sq{b}', f'z1{b}', f'zt{b}', f'ktok{b}', f'rr{b}'
            kpA, kpB, kpT = f'ps{3*b}', f'ps{3*b+1}', f'ps{3*b+2}'
            S.dma('pool', xb.rearrange("p a b -> p (a b)"), xT_d[s], writes=[kx], name=kx)
            S.op('act', lambda e, o=sq, i=xb: e.activation(out=o, in_=i, func=AF.Square), reads=[kx], writes=[ksq])
            for kc in range(8):
                S.op('pe', lambda e, kc=kc, o=pB, l=sq: e.matmul(o[:, 256:257], lhsT=l[:, kc, :], rhs=onesb[:, 0:1],
                                                                start=(kc == 0), stop=(kc == 7)),
                     reads=[ksq, 'onesb'], writes=[kpB], signal=False)
            for kc in range(8):
                S.op('pe', lambda e, kc=kc, o=pA, l=xb: e.matmul(o[:, 0:512], lhsT=l[:, kc, :], rhs=Wkv[:, kc, 0:512],
                                                                start=(kc == 0), stop=(kc == 7)),
                     reads=[kx, 'Wkv'], writes=[kpA], signal=False)
            for kc in range(8):
                S.op('pe', lambda e, kc=kc, o=pB, l=xb: e.matmul(o[:, 0:256], lhsT=l[:, kc, :], rhs=Wkv[:, kc, 512:768],
                                                                start=(kc == 0), stop=(kc == 7)),
                     reads=[kx, 'Wkv'], writes=[kpB], signal=(kc == 7))
            if A_LEVEL < 1:
                continue
            rsqrt_act(rb[:, 0:1], pB[:, 256:257], 1.0 / 1024.0, [kpB, 'epsc'], [kr])
            rcol = rb[:, 0:1]
            if A_LEVEL < 2:
                continue
            S.op('act', lambda e, o=z1b, i=pA, r_=rcol: e.mul(out=o, in_=i[:, 0:256], mul=r_), reads=[kpA, kr], writes=[kz])
            S.op('act', lambda e, o=kt_, i=pA, r_=rcol: e.mul(out=o[:, 256:512], in_=i[:, 256:512], mul=r_),
                 reads=[kpA, kr], writes=[kk])
            S.op('act', lambda e, s=s, i=pB, r_=rcol: e.mul(out=Vaug[:, :, s, :, 0:64],
                                                          in_=i[:, 0:256].rearrange("p (k g d) -> p k g d", k=2, g=2), mul=r_),
                 reads=[kpB, kr], writes=[('V', s)])
            if s % 4 == 3:
                S.op('act', lambda e, s=s, r_=rcol: e.copy(out=rown[:, s // 4:s // 4 + 1], in_=r_), reads=[kr], writes=['rown'])
            if A_LEVEL < 3:
                continue
            S.op('dve', lambda e, o=ztb, i=z1b: e.tensor_tensor(out=o, in0=i, in1=i, op=ALU.mult), reads=[kz], writes=[kzt])
            S.op('dve', lambda e, o=rb, i=ztb: e.tensor_reduce(out=o[:, 4:8], in_=i.rearrange("p (a d) -> p a d", d=64),
                                                             axis=AX.X, op=ALU.add), reads=[kzt], writes=[kr + 'k'])
            rsqrt_act(rb[:, 4:8], rb[:, 4:8], 1.0 / 64.0, [kr + 'k', 'epsc'], [kr + 'k'])
            S.op('dve', lambda e, o=ztb, i=z1b, r_=rb: e.tensor_tensor(
                out=o.rearrange("p (a d) -> p a d", d=64), in0=i.rearrange("p (a d) -> p a d", d=64),
                in1=r_[:, 4:8].unsqueeze(2).to_broadcast([128, 4, 64]), op=ALU.mult), reads=[kz, kr + 'k'], writes=[kzt])
            S.op('dve', lambda e, o=kt_, i=ztb: e.tensor_tensor(
                out=o[:, 0:256].rearrange("p (k g d) -> p k g d", k=2, g=2),
                in0=i.rearrange("p (k g d) -> p k g d", k=2, g=2),
                in1=kg12.unsqueeze(2).to_broadcast([128, 2, 2, 64]), op=ALU.mult), reads=[kzt, 'c0'], writes=[kk])
            if A_LEVEL < 4:
                continue
            for t4 in range(4):
                S.op('pe', lambda e, t4=t4, o=pT, i=kt_: e.transpose(o[:, t4 * 128:(t4 + 1) * 128], i[:, t4 * 128:(t4 + 1) * 128], identb),
                     reads=[kk, 'identb'], writes=[kpT], signal=(t4 == 3))
            if A_LEVEL < 5:
                continue
            S.op('dve', lambda e, s=s, i=pT: e.tensor_copy(out=Kst[:, :, s * 128:(s + 1) * 128],
                                                         in_=i[:, 0:256].rearrange("p (a t) -> p a t", t=128)),
                 reads=[kpT], writes=[('K', s)])
            if A_LEVEL < 6:
                continue
            S.op('dve', lambda e, s=s, i=pT: e.tensor_copy(out=KCraw[:, :, s * 128:(s + 1) * 128],
                                                  in_=i[:, 256:512].rearrange("p (a t) -> p a t", t=128)),
                 reads=[kpT], writes=[('KC', s)])
        S.barrier()
        if debug:
            dbg("Ksel0", Kst[:, 0, 3 * 128:4 * 128], 128)
            dbg("Kwin0", Kst[:, 1, 3 * 128:4 * 128], 128)
            dbg("Vsel0", Vaug[:, 0, 3, :, :].rearrange("p g d -> p (g d)"), 130)
            dbg("KCraw0", KCraw[:, 0, 3 * 128:4 * 128], 128)
        if stop_after == 'A':
            return _finish()

        chid = A.alloc([2], F32)
        gT = A.alloc([512], BF16)
        kc32 = A.alloc([4, 64], F32)
        kcsq = A.alloc([4, 64], F32)
        kcn = A.alloc([4, 2, 64], BF16)
        rcs = A.alloc([4], F32)
        for kind in range(2):
            for l in range(32):
                S.op('pe', lambda e, kind=kind, l=l: e.matmul(P[7][:, kind:kind + 1], lhsT=w1s[0:64, kind, l, :],
                                                              rhs=posTb[0:64, kind, l:l + 1], start=(l == 0), stop=(l == 31)),
                     reads=['w1s', 'posTb'], writes=['ps7'], signal=(l == 31))
        S.op('dve', lambda e: e.tensor_tensor(out=chid, in0=P[7][:, 0:2], in1=b1c, op=ALU.add), reads=['ps7', 'b1c'], writes=['chid'])
        for kind in range(2):
            for g in range(2):
                hp = P[(kind * 2 + g) % 2]
                khp = f'ps{(kind * 2 + g) % 2}'
                for l in range(32):
                    base = 0 if l < 16 else 16
                    rhs = KCraw[g * 64:(g + 1) * 64, kind, base:base + 8192].rearrange("p (c s) -> p s c", s=16)[:, l - base, :]
                    S.op('pe', lambda e, kind=kind, g=g, l=l, rhs=rhs, hp=hp: e.matmul(
                        hp, lhsT=w1s[g * 64:(g + 1) * 64, kind, l, :], rhs=rhs, start=(l == 0), stop=(l == 31)),
                         reads=['w1s'], writes=[khp], signal=(l == 31))
                S.op('act', lambda e, kind=kind, hp=hp: e.activation(out=gT, in_=hp, func=AF.Gelu_apprx_tanh, bias=chid[:, kind:kind + 1]),
                     reads=[khp, 'chid'], writes=['gT'])
                cp = P[2 + (kind * 2 + g) % 2]
                kcp = f'ps{2 + (kind * 2 + g) % 2}'
                for ct in range(4):
                    S.op('pe', lambda e, kind=kind, ct=ct, cp=cp: e.matmul(cp[:, ct * 64:(ct + 1) * 64], lhsT=gT[:, ct * 128:(ct + 1) * 128],
                                                                         rhs=w2b[:, kind, :], start=True, stop=True),
                         reads=['gT', 'w2b'], writes=[kcp], signal=(ct == 3))
                cpv = cp[:, 0:256].rearrange("p (c d) -> p c d", d=64)
                if kind == 0:
                    S.op('dve', lambda e, cpv=cpv: e.tensor_tensor(out=kc32, in0=cpv, in1=b2k.unsqueeze(1).to_broadcast([128, 4, 64]), op=ALU.add),
                         reads=[kcp, 'c0'], writes=['kc32'])
                    S.op('dve', lambda e: e.tensor_tensor(out=kcsq, in0=kc32, in1=kc32, op=ALU.mult), reads=['kc32'], writes=['kcsq'])
                    S.op('dve', lambda e: e.tensor_reduce(out=rcs, in_=kcsq, axis=AX.X, op=ALU.add), reads=['kcsq'], writes=['rcs'])
                    rsqrt_act(rcs, rcs, 1.0 / 64.0, ['rcs', 'epsc'], ['rcs'])
                    S.op('dve', lambda e: e.tensor_tensor(out=kcsq, in0=kc32, in1=rcs.unsqueeze(2).to_broadcast([128, 4, 64]), op=ALU.mult),
                         reads=['kc32', 'rcs'], writes=['kcsq'])
                    S.op('dve', lambda e, g=g: e.tensor_tensor(out=kcn[:, :, g, :], in0=kcsq, in1=kg0.unsqueeze(1).to_broadcast([128, 4, 64]), op=ALU.mult),
                         reads=['kcsq', 'c0'], writes=['kcn'])
                else:
                    S.op('dve', lambda e, cpv=cpv: e.tensor_tensor(out=kc32, in0=cpv, in1=b2v.unsqueeze(1).to_broadcast([128, 4, 64]), op=ALU.add),
                         reads=[kcp, 'c0'], writes=['kc32'])
                    S.op('dve', lambda e, g=g: e.tensor_tensor(out=Rc[:, :, g, 0:64], in0=kc32, in1=cvalid.unsqueeze(2).to_broadcast([128, 4, 64]), op=ALU.mult),
                         reads=['kc32', 'c0'], writes=['Rc'])
            if kind == 0:
                for ct in range(4):
                    S.op('pe', lambda e, ct=ct: e.transpose(PB[4][:, ct * 128:(ct + 1) * 128], kcn[:, ct, :, :].rearrange("p g d -> p (g d)"), identb),
                         reads=['kcn', 'identb'], writes=['ps4'], signal=(ct == 3))
                S.op('dve', lambda e: e.tensor_copy(out=KcT, in_=PB[4][:, 0:512]), reads=['ps4'], writes=['KcT'])
        S.barrier()
        if debug:
            dbg("KcT", KcT, 512)
            dbg("Rc0", Rc[:, 0, :, :].rearrange("p g d -> p (g d)"), 386)
        if stop_after == 'B':
            return _finish()
        A.release(base_top)

        mixedT = A.alloc([8, 2048], BF16)
        QT = A.alloc([16, 4, 128], BF16)
        gsig = A.alloc([16, 24], F32)
        mark2 = A.mark()
        Wr = A.alloc([8, 1560], BF16)
        g1c2 = A.alloc([8], F32)
        wsTm = A.alloc([8, 128], BF16)
        bsT = A.alloc([8], F32)
        xb2 = [A.alloc([8, 128], BF16) for _ in range(2)]
        zq = A.alloc([512], F32)
        zq2 = A.alloc([512], F32)
        qn = A.alloc([512], BF16)
        u32 = A.alloc([512], F32)
        v32 = A.alloc([512], F32)
        vn = A.alloc([512], BF16)
        og_off = A.mark()
        og = A.alloc([512], F32)
        og2 = A.alloc([512], F32)
        wsT32 = A.view(og_off, [8, 128], F32)
        mg = A.alloc([512], BF16)
        st8 = A.alloc([16], F32)
        Wr32 = [A.alloc([1560], F32)]

        S.dma('sp', g1c2, g1_d, writes=['g1c2'], name='c2')
        S.dma('sp', wsT32.rearrange("p a b -> p (a b)"), wsT_d, writes=['wsT32'], name='c2')
        S.dma('sp', bsT, bsT_d, writes=['bsT'], name='c2')
        S.op('dve', lambda e: e.tensor_tensor(out=wsTm, in0=wsT32, in1=trim[:, 0, :].unsqueeze(1).to_broadcast([128, 8, 128]), op=ALU.mult),
             reads=['wsT32'], writes=['wsTm'])
        for kc in range(8):
            wb = Wr32[0]
            S.dma('sp', wb, w_in_d[:, kc, 768:2328], writes=['Wr320'], name='Wr320')
            S.op('dve', lambda e, kc=kc, wb=wb: e.tensor_scalar(out=Wr[:, kc, :], in0=wb, scalar1=g1c2[:, kc:kc + 1], scalar2=None, op0=ALU.mult),
                 reads=['Wr320', 'g1c2'], writes=['Wr'])

        for i in range(NOWN):
            xb = xb2[i % 2]
            kx = f'xb2{i%2}'
            S.dma('pool', xb.rearrange("p a b -> p (a b)"), xT_d[4 * i + 3], writes=[kx], name=kx)
            specs = [(0, 0, 512, 0), (1, 0, 24, 512), (2, 0, 512, 536), (3, 0, 512, 1048)]
            for (bk, o0, n, c0) in specs:
                for kc in range(8):
                    S.op('pe', lambda e, bk=bk, n=n, c0=c0, kc=kc, xb=xb: e.matmul(P[bk][:, 0:n], lhsT=xb[:, kc, :], rhs=Wr[:, kc, c0:c0 + n],
                                                                            start=(kc == 0), stop=(kc == 7)),
                         reads=[kx, 'Wr'], writes=[f'ps{bk}'], signal=(kc == 7))
            rcol = rown[:, i:i + 1]
            S.op('act', lambda e, r_=rcol: e.mul(out=zq, in_=P[0], mul=r_), reads=['ps0'], writes=['zq'])
            S.op('act', lambda e, i=i, r_=rcol: e.activation(out=gsig[:, i, :], in_=P[1][:, 0:24], func=AF.Sigmoid, scale=r_),
                 reads=['ps1'], writes=['gsig'])
            S.op('act', lambda e, r_=rcol: e.activation(out=u32, in_=P[2], func=AF.Gelu_apprx_tanh, scale=r_), reads=['ps2'], writes=['u32'])
            S.op('act', lambda e, r_=rcol: e.activation(out=v32, in_=P[3], func=AF.Gelu_apprx_tanh, scale=r_), reads=['ps3'], writes=['v32'])
            S.op('dve', lambda e: e.tensor_tensor(out=zq2, in0=zq, in1=zq, op=ALU.mult), reads=['zq'], writes=['zq2'])
            S.op('dve', lambda e: e.tensor_reduce(out=st8[:, 0:8], in_=zq2.rearrange("p (h d) -> p h d", d=64), axis=AX.X, op=ALU.add),
                 reads=['zq2'], writes=['ssq'])
            rsqrt_act(st8[:, 0:8], st8[:, 0:8], 1.0 / 64.0, ['ssq'], ['ssq'])
            S.op('dve', lambda e: e.tensor_tensor(out=zq2.rearrange("p (h d) -> p h d", d=64), in0=zq.rearrange("p (h d) -> p h d", d=64),
                                                  in1=st8[:, 0:8].unsqueeze(2).to_broadcast([128, 8, 64]), op=ALU.mult),
                 reads=['zq', 'ssq'], writes=['zq2'])
            S.op('dve', lambda e: e.tensor_tensor(out=qn.rearrange("p (r g d) -> p g r d", r=4, g=2),
                                                  in0=zq2.rearrange("p (g r d) -> p g r d", g=2, r=4),
                                                  in1=qg8.unsqueeze(1).unsqueeze(1).to_broadcast([128, 2, 4, 64]), op=ALU.mult),
                 reads=['zq2', 'qg8'], writes=['qn'])
            for r4 in range(4):
                S.op('pe', lambda e, r4=r4: e.transpose(PB[4][:, r4 * 128:(r4 + 1) * 128], qn[:, r4 * 128:(r4 + 1) * 128], identb),
                     reads=['qn'], writes=['ps4'], signal=(r4 == 3))
            S.op('dve', lambda e, i=i: e.tensor_copy(out=QT[:, i, :, :].rearrange("p r q -> p (r q)"), in_=PB[4][:, 0:512]),
                 reads=['ps4'], writes=['QT'])
            S.op('dve', lambda e: e.tensor_tensor(out=og, in0=v32, in1=v32, op=ALU.mult), reads=['v32'], writes=['og'])
            S.op('dve', lambda e: e.tensor_reduce(out=st8[:, 8:9], in_=og, axis=AX.X, op=ALU.add), reads=['og'], writes=['ssv'])
            rsqrt_act(st8[:, 8:9], st8[:, 8:9], 1.0 / 512.0, ['ssv'], ['ssv'])
            S.op('dve', lambda e: e.scalar_tensor_tensor(out=vn, in0=v32, scalar=st8[:, 8:9], in1=gmv, op0=ALU.mult, op1=ALU.mult),
                 reads=['v32', 'ssv'], writes=['vn'])
            for g8 in range(8):
                S.op('pe', lambda e, g8=g8: e.matmul(P[5][:, g8 * 64:(g8 + 1) * 64], lhsT=wsTm[:, g8, :], rhs=vn[:, g8 * 64:(g8 + 1) * 64],
                                                     start=True, stop=True),
                     reads=['vn', 'wsTm'], writes=['ps5'], signal=(g8 == 7))
            S.op('dve', lambda e: e.tensor_tensor(out=og.rearrange("p (g d) -> p g d", d=64), in0=P[5].rearrange("p (g d) -> p g d", d=64),
                                                  in1=bsT.unsqueeze(2).to_broadcast([128, 8, 64]), op=ALU.add),
                 reads=['ps5', 'bsT'], writes=['og'])
            S.op('dve', lambda e: e.tensor_tensor(out=og, in0=og, in1=u32, op=ALU.mult), reads=['og', 'u32'], writes=['og'])
            S.op('dve', lambda e: e.tensor_tensor(out=og2, in0=og, in1=og, op=ALU.mult), reads=['og'], writes=['og2'])
            S.op('dve', lambda e: e.tensor_reduce(out=st8[:, 9:10], in_=og2, axis=AX.X, op=ALU.add), reads=['og2'], writes=['sso'])
            rsqrt_act(st8[:, 9:10], st8[:, 9:10], 1.0 / 512.0, ['sso'], ['sso'])
            S.op('dve', lambda e: e.scalar_tensor_tensor(out=mg, in0=og, scalar=st8[:, 9:10], in1=ong, op0=ALU.mult, op1=ALU.mult),
                 reads=['og', 'sso'], writes=['mg'])
            for c in range(4):
                S.op('pe', lambda e, c=c: e.transpose(PB[6][:, c * 128:(c + 1) * 128], mg[:, c * 128:(c + 1) * 128], identb),
                     reads=['mg'], writes=['ps6'], signal=(c == 3))
            S.op('dve', lambda e, i=i: e.tensor_copy(out=mixedT[:, 4:8, i * 128:(i + 1) * 128], in_=PB[6][:, 0:512].rearrange("p (c t) -> p c t", t=128)),
                 reads=['ps6'], writes=['mixedT'])
        S.barrier()
        if debug:
            dbg("QT0", QT[:, 0, 0, :], 128)
            dbg("gsig", gsig.rearrange("p a b -> p (a b)"), 384)
            dbg("mgm0", mixedT[:, 4, 0:128], 128)
        if stop_after == 'A2':
            return _finish()
        A.release(mark2)

        am = [A.alloc([128], F32) for _ in range(2)]
        eT = [A.alloc([4, 128], BF16) for _ in range(3)]
        Pm = [A.alloc([4, 128], BF16) for _ in range(3)]
        oacc = A.alloc([8, 64], F32)
        otmp = A.alloc([4, 64], F32)
        osq = A.alloc([512], F32)
        imp = A.alloc([128], F32)
        imp2 = A.alloc([128], F32)
        m8 = A.alloc([16], F32)
        thr = A.alloc([2], F32)
        selb = [A.alloc([128], BF16) for _ in range(2)]
        selX = A.alloc([128, 64], BF16)
        rz = A.alloc([4], F32)
        gz = A.alloc([4], F32)
        ssa = A.alloc([2], F32)
        ma = A.alloc([512], BF16)
        gs4 = gsig.rearrange("p i (h k) -> p i h k", k=3)

        ring = [0]

        def score_tile(lhsT, rhs, bias_fn, g):
            n = ring[0]
            ring[0] += 1
            sp_, ksp = P[n % 2], f'ps{n % 2}'
            e_, ke = eT[n % 3], f'eT{n % 3}'
            S.op('pe', lambda e, sp_=sp_, lhsT=lhsT, rhs=rhs: e.matmul(sp_, lhsT=lhsT, rhs=rhs, start=True, stop=True),
                 reads=[], writes=[ksp])
            for r in range(4):
                S.op('act', lambda e, r=r, sp_=sp_, e_=e_, b_=bias_fn(4 * g + r): e.activation(
                    out=e_[:, r, :], in_=sp_[:, r * 128:(r + 1) * 128], func=AF.Exp, bias=b_),
                     reads=[ksp], writes=[ke])
            return n, e_, ke

        def qblock(i):
            sq_ = 4 * i + 3
            amt, kam = am[i % 2], f'am{i%2}'
            S.dma('sp', amt, addmask_d[:, i * 128:(i + 1) * 128], writes=[kam], name=kam)
            def grp(g):
                QTg = QT[g * 64:(g + 1) * 64, i, :, :].rearrange("p r q -> p (r q)")
                nct = i // 4 + 1
                U5 = P[5].rearrange("p (r c) -> p r c", c=256)
                U6 = P[6].rearrange("p (r c) -> p r c", c=256)
                Ur = [U5[:, 0, :], U5[:, 1, :], U6[:, 0, :], U6[:, 1, :]]
                for ct in range(nct):
                    n, e_, ke = score_tile(KcT[g * 64:(g + 1) * 64, ct * 128:(ct + 1) * 128], QTg,
                                           lambda h, ct=ct: biasC[:, i, ct, h:h + 1], g)
                    if ct == nct - 1:
                        S.op('pool', lambda e, e_=e_: e.tensor_tensor(out=e_, in0=e_, in1=cmaskT[:, i, :].unsqueeze(1).to_broadcast([128, 4, 128]),
                                                                     op=ALU.mult), reads=[ke], writes=[ke])
                    for r in range(4):
                        S.op('pe', lambda e, r=r, ct=ct, e_=e_: e.matmul(Ur[r][:, 0:193], lhsT=e_[:, r, :], rhs=Rc[:, ct, g, :],
                                                                       start=(ct == 0 and r in (0, 2)), stop=(ct == nct - 1 and r in (1, 3))),
                             reads=[ke], writes=['ps5' if r < 2 else 'ps6'], signal=(r == 3))
                S.op('dve', lambda e: e.tensor_scalar(out=rz[:, 0:2].unsqueeze(2), in0=U5[:, :, 64:65], scalar1=1e-30, scalar2=None, op0=ALU.max),
                     reads=['ps5'], writes=['rz'])
                S.op('dve', lambda e: e.tensor_scalar(out=rz[:, 2:4].unsqueeze(2), in0=U6[:, :, 64:65], scalar1=1e-30, scalar2=None, op0=ALU.max),
                     reads=['ps6'], writes=['rz'])
                S.op('dve', lambda e: e.reciprocal(out=rz, in_=rz), reads=['rz'], writes=['rz'])
                S.op('dve', lambda e, g=g: e.tensor_tensor(out=gz, in0=rz, in1=gs4[:, i, 4 * g:4 * g + 4, 0], op=ALU.mult), reads=['rz'], writes=['gz'])
                S.op('dve', lambda e, g=g: e.tensor_tensor(out=oacc[:, 4 * g:4 * g + 2, :], in0=U5[:, :, 0:64],
                                                           in1=gz[:, 0:2].unsqueeze(2).to_broadcast([128, 2, 64]), op=ALU.mult),
                     reads=['ps5', 'gz'], writes=['oacc'])
                S.op('dve', lambda e, g=g: e.tensor_tensor(out=oacc[:, 4 * g + 2:4 * g + 4, :], in0=U6[:, :, 0:64],
                                                           in1=gz[:, 2:4].unsqueeze(2).to_broadcast([128, 2, 64]), op=ALU.mult),
                     reads=['ps6', 'gz'], writes=['oacc'])
                for r in range(4):
                    S.op('dve', lambda e, r=r: e.scalar_tensor_tensor(out=imp, in0=Ur[r][:, 65:193], scalar=rz[:, r:r + 1],
                                                                      in1=(amt if r == 0 else imp), op0=ALU.mult, op1=ALU.add),
                         reads=['ps5' if r < 2 else 'ps6', 'rz', kam, 'imp'], writes=['imp'])
                S.op('dve', lambda e: e.max(out=m8[:, 0:8], in_=imp), reads=['imp'], writes=['m8a'])
                S.op('dve', lambda e: e.match_replace(out=imp2, in_to_replace=m8[:, 0:8], in_values=imp, imm_value=-3.0e38),
                     reads=['imp', 'm8a'], writes=['imp2'])
                S.op('dve', lambda e: e.max(out=m8[:, 8:16], in_=imp2), reads=['imp2'], writes=['m8b'])
                S.op('dve', lambda e: e.tensor_scalar(out=thr[:, 0:1], in0=m8[:, 15:16], scalar1=-1.0e29, scalar2=None, op0=ALU.max),
                     reads=['m8b'], writes=['thr'])
                sel_, ksel = selb[g], f'sel{g}'
                S.op('dve', lambda e, sel_=sel_: e.tensor_scalar(out=sel_, in0=imp, scalar1=thr[:, 0:1], scalar2=None, op0=ALU.is_ge),
                     reads=['imp', 'thr'], writes=[ksel])
                nb = 2 * (sq_ + 1)
                S.op('pool', lambda e, sel_=sel_: e.tensor_copy(out=selX[:, 0:nb, :], in_=sel_[:, 0:nb].unsqueeze(2).to_broadcast([128, nb, 64])),
                     reads=[ksel], writes=['selX'])
                for br in (1, 2):
                    ob, kob = (P[7], 'ps7') if br == 1 else (P[4], 'ps4')
                    O4 = ob.rearrange("p (r c) -> p r c", c=128)
                    kts = list(range(0, sq_ + 1)) if br == 1 else list(range(max(0, sq_ - 4), sq_ + 1))
                    for kt in kts:
                        n, e_, ke = score_tile(Kst[g * 64:(g + 1) * 64, br - 1, kt * 128:(kt + 1) * 128], QTg,
                                               lambda h, kt=kt: biasT[:, sq_ - kt, h:h + 1], g)
                        src, ksrc = e_, ke
                        if br == 1:
                            pmv = PB[2][:, (n % 4) * 128:(n % 4 + 1) * 128]
                            kpm = f'pm{n % 4}'
                            S.op('pe', lambda e, kt=kt, pmv=pmv: e.transpose(
                                pmv, selX[:, 2 * kt:2 * kt + 2, :].rearrange("p a b -> p (a b)"), identb),
                                 reads=['selX'], writes=[kpm])
                            p_, kp = Pm[n % 3], f'Pm{n % 3}'
                            S.op('dve', lambda e, p_=p_, e_=e_, pmv=pmv: e.tensor_tensor(
                                out=p_, in0=e_, in1=pmv.unsqueeze(1).to_broadcast([128, 4, 128]), op=ALU.mult),
                                 reads=[ke, kpm], writes=[kp])
                            src, ksrc = p_, kp
                            if kt == sq_:
                                S.op('pool', lambda e, p_=p_: e.tensor_tensor(out=p_, in0=p_, in1=trim[:, 0, :].unsqueeze(1).to_broadcast([128, 4, 128]),
                                                                             op=ALU.mult), reads=[kp], writes=[kp])
                        else:
                            if kt == sq_ or kt == sq_ - 4:
                                mi = 0 if kt == sq_ else 1
                                S.op('pool', lambda e, e_=e_, mi=mi: e.tensor_tensor(out=e_, in0=e_, in1=trim[:, mi, :].unsqueeze(1).to_broadcast([128, 4, 128]),
                                                                                   op=ALU.mult), reads=[ke], writes=[ke])
                        for r in range(4):
                            S.op('pe', lambda e, r=r, kt=kt, src=src, O4=O4, br=br, first=(kt == kts[0] and r == 0), last=(kt == kts[-1] and r == 3): e.matmul(
                                O4[:, r, 0:65], lhsT=src[:, r, :], rhs=Vaug[:, br - 1, kt, g, :], start=first, stop=last),
                                 reads=[ksrc], writes=[kob], signal=(r == 3))
                    S.op('dve', lambda e, O4=O4: e.tensor_scalar(out=rz.unsqueeze(2), in0=O4[:, :, 64:65], scalar1=1e-30, scalar2=None, op0=ALU.max),
                         reads=[kob], writes=['rz'])
                    S.op('dve', lambda e: e.reciprocal(out=rz, in_=rz), reads=['rz'], writes=['rz'])
                    S.op('dve', lambda e, g=g, br=br: e.tensor_tensor(out=gz, in0=rz, in1=gs4[:, i, 4 * g:4 * g + 4, br], op=ALU.mult),
                         reads=['rz'], writes=['gz'])
                    S.op('dve', lambda e, O4=O4: e.tensor_tensor(out=otmp, in0=O4[:, :, 0:64], in1=gz.unsqueeze(2).to_broadcast([128, 4, 64]), op=ALU.mult),
                         reads=[kob, 'gz'], writes=['otmp'])
                    S.op('pool', lambda e, g=g: e.tensor_tensor(out=oacc[:, 4 * g:4 * g + 4, :], in0=oacc[:, 4 * g:4 * g + 4, :], in1=otmp, op=ALU.add),
                         reads=['otmp', 'oacc'], writes=['oacc'])
            grp(0)
            grp(1)
            oflat = oacc.rearrange("p h d -> p (h d)")
            if debug and i in (0, 5):
                dbg(f"oattn{i}", oflat, 512, reads=['oacc'])
            S.op('dve', lambda e: e.tensor_tensor(out=osq, in0=oflat, in1=oflat, op=ALU.mult), reads=['oacc'], writes=['osq'])
            S.op('dve', lambda e: e.tensor_reduce(out=ssa[:, 0:1], in_=osq, axis=AX.X, op=ALU.add), reads=['osq'], writes=['ssa'])
            rsqrt_act(ssa[:, 0:1], ssa[:, 0:1], 1.0 / 512.0, ['ssa'], ['ssa'])
            S.op('dve', lambda e: e.scalar_tensor_tensor(out=ma, in0=oflat, scalar=ssa[:, 0:1], in1=ona, op0=ALU.mult, op1=ALU.mult),
                 reads=['oacc', 'ssa'], writes=['ma'])
            for c in range(4):
                S.op('pe', lambda e, c=c: e.transpose(PB[3][:, c * 128:(c + 1) * 128], ma[:, c * 128:(c + 1) * 128], identb),
                     reads=['ma'], writes=['ps3'], signal=(c == 3))
            S.op('dve', lambda e, i=i: e.tensor_copy(out=mixedT[:, 0:4, i * 128:(i + 1) * 128], in_=PB[3][:, 0:512].rearrange("p (c t) -> p c t", t=128)),
                 reads=['ps3'], writes=['mixedT'])
        for i in range(NOWN):
            qblock(i)
        S.barrier()
        if stop_after == 'C':
            return _finish()

        mixed_off = base_top
        assert mixed_off == 88000, mixed_off
        A.top = 0
        acc = A.alloc([16, 1024], F32)
        gmoe = A.alloc([16, 32], F32)
        bgu = A.alloc([NE * 16], F32)
        dA = A.mark()
        identf2 = A.alloc([128], F32)
        identb2 = A.alloc([128], BF16)
        epsD = A.alloc([8], F32)
        eps_holder[0] = epsD[:, 0:1]
        rowD = A.alloc([NROWD], F32)
        wr32 = A.alloc([8, 32], F32)
        wrh = A.alloc([8, 32], BF16)
        wrl = A.alloc([8, 32], BF16)
        std = A.alloc([64], F32)
        lgt = A.alloc([32], F32)
        msk = A.alloc([32], F32)
        ex = A.alloc([32], F32)
        assert A.top <= mixed_off, A.top
        A.top = mixed_off + 32768
        xnT = A.alloc([8, 2048], BF16)
        dB = A.mark()
        wout = A.alloc([8, 1024], BF16)
        xo = [A.alloc([1024], F32) for _ in range(2)]
        xn = A.alloc([1024], F32)
        xh = A.alloc([1024], BF16)
        xl = A.alloc([1024], BF16)
        loT = A.alloc([8, 128], BF16)
        g2bc = rowD[:, 0:1024]
        brow = rowD[:, 1024:1056]

        S.op('pool', lambda e: e.memset(identf2, 1.0), writes=['identf2'])
        S.op('pool', lambda e: e.affine_select(out=identf2, in_=identf2, pattern=[[-1, 128]], compare_op=ALU.is_equal,
                                              fill=0.0, base=0, channel_multiplier=1), reads=['identf2'], writes=['identf2'])
        S.op('dve', lambda e: e.tensor_copy(out=identb2, in_=identf2), reads=['identf2'], writes=['identb2'])
        S.op('dve', lambda e: e.memset(epsD, EPS), writes=['epsD'])
        S.dma('sp', rowD, rowD_d[0:1, :].to_broadcast([128, NROWD]), writes=['d0'], name='d0')
        S.dma('sp', bgu, bgu_d, writes=['d0'], name='d0')
        S.dma('sp', wr32.rearrange("p a b -> p (a b)"), w_router_d, writes=['d0'], name='d0')
        S.dma('pool', wout.rearrange("p a b -> p (a b)"), w_out_d, writes=['d0p'], name='d0p')
        S.op('dve', lambda e: e.tensor_copy(out=wrh, in_=wr32), reads=['d0'], writes=['wrh'])
        S.op('dve', lambda e: e.tensor_tensor(out=wrl, in0=wr32, in1=wrh, op=ALU.subtract), reads=['d0', 'wrh'], writes=['wrl'])

        def dpre(i):
            xot, kxo = xo[i % 2], f'xo{i%2}'
            S.dma('sp', xot, xown_d[i], writes=[kxo], name=kxo)
            for half in range(2):
                for kc in range(8):
                    S.op('pe', lambda e, half=half, kc=kc: e.matmul(P[half], lhsT=mixedT[:, kc, i * 128:(i + 1) * 128],
                                                                   rhs=wout[:, kc, half * 512:(half + 1) * 512], start=(kc == 0), stop=(kc == 7)),
                         reads=['d0p'], writes=[f'ps{half}'], signal=(kc == 7))
                S.op('dve', lambda e, half=half: e.tensor_tensor(out=acc[:, i, half * 512:(half + 1) * 512], in0=P[half],
                                                                 in1=xot[:, half * 512:(half + 1) * 512], op=ALU.add),
                     reads=[f'ps{half}', kxo], writes=[('acc', i)])
            S.op('dve', lambda e: e.tensor_tensor(out=xn, in0=acc[:, i, :], in1=acc[:, i, :], op=ALU.mult), reads=[('acc', i)], writes=['xn'])
            S.op('dve', lambda e: e.tensor_reduce(out=std[:, 0:1], in_=xn, axis=AX.X, op=ALU.add), reads=['xn'], writes=['ss2'])
            rsqrt_act(std[:, 0:1], std[:, 0:1], 1.0 / 1024.0, ['ss2', 'epsD'], ['ss2'])
            S.op('dve', lambda e: e.scalar_tensor_tensor(out=xn, in0=acc[:, i, :], scalar=std[:, 0:1], in1=g2bc, op0=ALU.mult, op1=ALU.mult),
                 reads=[('acc', i), 'ss2', 'd0'], writes=['xn'])
            if D_LEVEL < 2:
                return
            S.op('dve', lambda e: e.tensor_copy(out=xh, in_=xn), reads=['xn'], writes=['xh'])
            S.op('dve', lambda e: e.tensor_tensor(out=xl, in0=xn, in1=xh, op=ALU.subtract), reads=['xn', 'xh'], writes=['xl'])
            for c in range(8):
                S.op('pe', lambda e, c=c: e.transpose(PB[2][:, c * 128:(c + 1) * 128], xh[:, c * 128:(c + 1) * 128], identb2),
                     reads=['xh', 'identb2'], writes=['ps2'], signal=(c == 7))
            for c in range(8):
                S.op('pe', lambda e, c=c: e.transpose(PB[3][:, c * 128:(c + 1) * 128], xl[:, c * 128:(c + 1) * 128], identb2),
                     reads=['xl', 'identb2'], writes=['ps3'], signal=(c == 7))
            S.op('dve', lambda e: e.tensor_copy(out=xnT[:, :, i * 128:(i + 1) * 128], in_=PB[2].rearrange("p (c t) -> p c t", t=128)),
                 reads=['ps2'], writes=[('xnT', i)])
            S.op('dve', lambda e: e.tensor_copy(out=loT, in_=PB[3].rearrange("p (c t) -> p c t", t=128)), reads=['ps3'], writes=['loT'])
            if D_LEVEL < 3:
                return
            nmm = 0
            for (lt, kl, wt, kw_) in ((0, ('xnT', i), wrh, 'wrh'), (1, 'loT', wrh, 'wrh'), (0, ('xnT', i), wrl, 'wrl')):
                for kc in range(8):
                    lhs = xnT[:, kc, i * 128:(i + 1) * 128] if lt == 0 else loT[:, kc, :]
                    S.op('pe', lambda e, lhs=lhs, wt=wt, kc=kc, first=(nmm == 0), last=(nmm == 23): e.matmul(
                        P[4][:, 0:32], lhsT=lhs, rhs=wt[:, kc, :], start=first, stop=last),
                         reads=[kl, kw_], writes=['ps4'], signal=(nmm == 23))
                    nmm += 1
            S.op('dve', lambda e: e.tensor_tensor(out=lgt, in0=P[4][:, 0:32], in1=brow, op=ALU.add), reads=['ps4', 'd0'], writes=['lgt'])
            S.op('dve', lambda e: e.max(out=std[:, 8:16], in_=lgt), reads=['lgt'], writes=['m8r'])
            S.op('dve', lambda e: e.tensor_scalar(out=msk, in0=lgt, scalar1=std[:, 11:12], scalar2=None, op0=ALU.is_ge), reads=['lgt', 'm8r'], writes=['msk'])
            S.op('dve', lambda e: e.tensor_scalar(out=std[:, 16:17], in0=std[:, 8:9], scalar1=-1.0, scalar2=None, op0=ALU.mult), reads=['m8r'], writes=['negm'])
            S.op('act', lambda e: e.activation(out=ex, in_=lgt, func=AF.Exp, bias=std[:, 16:17]), reads=['lgt', 'negm'], writes=['ex'])
            S.op('dve', lambda e: e.tensor_tensor(out=ex, in0=ex, in1=msk, op=ALU.mult), reads=['ex', 'msk'], writes=['ex'])
            S.op('dve', lambda e: e.tensor_reduce(out=std[:, 17:18], in_=ex, axis=AX.X, op=ALU.add), reads=['ex'], writes=['se'])
            S.op('dve', lambda e: e.reciprocal(out=std[:, 17:18], in_=std[:, 17:18]), reads=['se'], writes=['se'])
            S.op('dve', lambda e: e.tensor_scalar(out=gmoe[:, i, :], in0=ex, scalar1=std[:, 17:18], scalar2=None, op0=ALU.mult),
                 reads=['ex', 'se'], writes=[('gmoe', i)])

        for i in range(NOWN):
            dpre(i)
        S.barrier()
        if debug:
            dbg("x1_0", acc[:, 0, :], 1024)
            dbg("gmoe", gmoe.rearrange("p a b -> p (a b)"), 512)
        if stop_after == 'Dpre':
            return _finish()

        A.top = dA
        t1 = [A.alloc([512], F32) for _ in range(2)]
        t2 = [A.alloc([512], F32) for _ in range(2)]
        bdrow = [A.alloc([1024], BF16) for _ in range(2)]
        ones128 = A.alloc([128], BF16)
        assert A.top <= mixed_off
        S.op('dve', lambda e: e.memset(ones128, 1.0), writes=['ones128'])
        A.top = mixed_off
        aT = A.alloc([8, 2048], BF16)
        A.top = dB
        Wd = A.alloc([8, 1024], BF16)
        Wg = [A.alloc([8, 2, 128], BF16) for _ in range(4)]

        cnt = [0, 0, 0]

        def expert(ex_):
            S.dma('pool', Wd.rearrange("p a b -> p (a b)"), wd_d[ex_], writes=['Wd'], name='Wd')
            bdr, kbd = bdrow[ex_ % 2], f'bdrow{ex_ % 2}'
            S.dma('pool', bdr[0:1, :], b_down_d[ex_:ex_ + 1, :], writes=[kbd], name=kbd)
            for m in range(8):
                wn = cnt[0]
                cnt[0] += 1
                wg, kwg = Wg[wn % 4], f'Wg{wn % 4}'
                S.dma('pool', wg.rearrange("p a b c -> p (a b c)"), wgu_d[ex_ * 8 + m], writes=[kwg], name=kwg)
                for tg in range(4):
                    n = cnt[1]
                    cnt[1] += 1
                    pg, pl = P[(2 * n) % 4], P[(2 * n + 1) % 4]
                    kpg, kpl = f'ps{(2 * n) % 4}', f'ps{(2 * n + 1) % 4}'
                    a1, a2 = t1[n % 2], t2[n % 2]
                    k1, k2 = f't1{n % 2}', f't2{n % 2}'
                    for gl, pp, kp in ((0, pg, kpg), (1, pl, kpl)):
                        for kc in range(8):
                            S.op('pe', lambda e, gl=gl, pp=pp, kc=kc, wg=wg, tg=tg: e.matmul(
                                pp, lhsT=wg[:, kc, gl, :], rhs=xnT[:, kc, tg * 512:(tg + 1) * 512], start=(kc == 0), stop=(kc == 7)),
                                 reads=[kwg], writes=[kp], signal=(kc == 7))
                    bcol = ex_ * 16 + m * 2
                    S.op('dve', lambda e, pg=pg, a1=a1, bcol=bcol: e.tensor_scalar(out=a1, in0=pg, scalar1=bgu[:, bcol:bcol + 1], scalar2=7.0,
                                                                                   op0=ALU.add, op1=ALU.min), reads=[kpg], writes=[k1])
                    S.op('act', lambda e, a1=a1, a2=a2: e.activation(out=a2, in_=a1, func=AF.Sigmoid, scale=1.702), reads=[k1], writes=[k2])
                    S.op('dve', lambda e, a1=a1, a2=a2: e.tensor_tensor(out=a1, in0=a1, in1=a2, op=ALU.mult), reads=[k1, k2], writes=[k1])
                    S.op('dve', lambda e, pl=pl, a2=a2, bcol=bcol: e.tensor_scalar(out=a2, in0=pl, scalar1=bgu[:, bcol + 1:bcol + 2], scalar2=7.0,
                                                                                   op0=ALU.add, op1=ALU.min), reads=[kpl, k1], writes=[k2])
                    S.op('dve', lambda e, a2=a2: e.tensor_scalar(out=a2, in0=a2, scalar1=-7.0, scalar2=1.0, op0=ALU.max, op1=ALU.add),
                         reads=[k2], writes=[k2])
                    S.op('dve', lambda e, a1=a1, a2=a2, m=m, tg=tg: e.tensor_tensor(out=aT[:, m, tg * 512:(tg + 1) * 512], in0=a1, in1=a2, op=ALU.mult),
                         reads=[k1, k2], writes=['aT'])
            for tt in range(16):
                for half in range(2):
                    k = cnt[2]
                    cnt[2] += 1
                    py, kpy = P[4 + k % 4], f'ps{4 + k % 4}'
                    for m in range(8):
                        S.op('pe', lambda e, py=py, m=m, tt=tt, half=half: e.matmul(
                            py, lhsT=aT[:, m, tt * 128:(tt + 1) * 128], rhs=Wd[:, m, half * 512:(half + 1) * 512], start=(m == 0), stop=False),
                             reads=['aT', 'Wd'], writes=[kpy], signal=False)
                    S.op('pe', lambda e, py=py, half=half: e.matmul(py, lhsT=ones128[0:1, :], rhs=bdr[0:1, half * 512:(half + 1) * 512],
                                                                   start=False, stop=True), reads=[kbd, 'ones128'], writes=[kpy])
                    S.op('dve', lambda e, py=py, tt=tt, half=half: e.scalar_tensor_tensor(
                        out=acc[:, tt, half * 512:(half + 1) * 512], in0=py, scalar=gmoe[:, tt, ex_:ex_ + 1],
                        in1=acc[:, tt, half * 512:(half + 1) * 512], op0=ALU.mult, op1=ALU.add),
                         reads=[kpy, ('acc', tt)], writes=[('acc', tt)])

        for ex_ in range(n_exp):
            expert(ex_)
        for i in range(NOWN):
            S.dma('sp', out_d[i], acc[:, i, :], reads=[('acc', i)], name='out')
        return _finish()


_CACHE = {}


def kernel(**inputs):
    inputs = {k: np.asarray(v) for k, v in inputs.items()}
    x = inputs["x"].astype(np.float32)
    W = _host_weights(inputs)
    if "nc" not in _CACHE:
        _CACHE["nc"] = build_program()
    nc, dbg_outs = _CACHE["nc"]
    in_maps = []
    for c in range(8):
        b, j = c // 4, c % 4
        pad = 3 - j
        xb = x[b]
        xp = np.concatenate([np.zeros((pad * 128, D), np.float32), xb], axis=0)[:8192]
        xT = np.ascontiguousarray(xp.reshape(64, 128, 8, 128).transpose(0, 3, 2, 1)).reshape(64, 128, 1024)
        xown = np.ascontiguousarray(xb.reshape(16, 4, 128, D)[:, j])
        m = dict(xT=xT, xown=xown)
        m.update(_host_consts(j))
        m.update(W)
        in_maps.append(m)
    res = run_bass_kernel_spmd(nc, in_maps, core_ids=list(range(8)))
    out = np.zeros((2, 8192, D), np.float32)
    ov = out.reshape(2, 16, 4, 128, D)
    for c in range(8):
        b, j = c // 4, c % 4
        ov[b, :, j] = res.results[c]["out"]
    _CACHE["last"] = res
    return out
```

```python
import numpy as np
from contextlib import ExitStack
import concourse.bass as bass
import concourse.mybir as mybir
from concourse.bass_utils import run_bass_kernel_spmd

F32 = mybir.dt.float32
BF16 = mybir.dt.bfloat16
AF = mybir.ActivationFunctionType
ALU = mybir.AluOpType
AX = mybir.AxisListType

ENG = ['pe', 'act', 'dve', 'pool', 'sp']
BLK = {'pe': 'tensor', 'act': 'scalar', 'dve': 'vector', 'pool': 'gpsimd', 'sp': 'sync'}


class Sched:
    def __init__(self, nc, stack):
        self.nc = nc
        self.stack = stack
        self.sem = {e: stack.enter_context(nc.semaphore("c_" + e)) for e in ENG}
        self.cnt = {e: 0 for e in ENG}
        self.prog = {e: [] for e in ENG}
        self.waited = {e: {} for e in ENG}
        self.lastw = {}
        self.readers = {}
        self.dsem = {}
        self.dcnt = {}
        self.dname = {}
        self.nops = {e: 0 for e in ENG}

    def _wait(self, eng, tok):
        sem, val, kind = tok
        if kind == 'dma':
            val = self.dcnt[self.dname[sem.num]]
        w = self.waited[eng]
        if w.get(sem.num, 0) >= val:
            return
        w[sem.num] = val
        self.prog[eng].append(lambda e, s=sem, v=val: e.wait_ge(s, v))

    def _deps(self, eng, reads, writes):
        for k in reads:
            t = self.lastw.get(k)
            if t is not None:
                self._wait(eng, t)
        for k in writes:
            t = self.lastw.get(k)
            if t is not None and (t[2] != eng or eng != 'pe'):
                self._wait(eng, t)
            for t in self.readers.get(k, {}).values():
                if t[2] != eng or eng != 'pe':
                    self._wait(eng, t)

    def _commit(self, tok, reads, writes):
        for k in reads:
            self.readers.setdefault(k, {})[tok[0].num] = tok
        for k in writes:
            self.lastw[k] = tok
            self.readers[k] = {}

    def op(self, eng, fn, reads=(), writes=(), signal=True):
        self._deps(eng, reads, writes)
        sem = self.sem[eng]
        self.nops[eng] += 1
        if signal:
            self.cnt[eng] += 1
            tok = (sem, self.cnt[eng], eng)
            self.prog[eng].append(lambda e, f=fn, s=sem: f(e).then_inc(s, 1))
        else:
            tok = (sem, self.cnt[eng] + 1, eng)
            self.prog[eng].append(lambda e, f=fn: f(e))
        self._commit(tok, reads, writes)

    def dma(self, q, out, in_, reads=(), writes=(), name=None, **kw):
        self._deps(q, reads, writes)
        if name not in self.dsem:
            self.dsem[name] = self.stack.enter_context(self.nc.semaphore("d_" + str(name)))
            self.dcnt[name] = 0
            self.dname[self.dsem[name].num] = name
        sem = self.dsem[name]
        self.dcnt[name] += 16
        tok = (sem, self.dcnt[name], 'dma')
        self.prog[q].append(
            lambda e, o=out, i=in_, s=sem, kw=kw: e.dma_start(out=o, in_=i, **kw).then_inc(s, 16))
        self._commit(tok, reads, writes)
        return tok

    def barrier(self):
        toks = [(self.sem[e], self.cnt[e], e) for e in ENG if self.cnt[e] > 0]
        toks += [(self.dsem[n], self.dcnt[n], 'dma') for n in self.dsem]
        for e in ENG:
            for t in toks:
                self._wait(e, t)

    def finish(self, eng='sp'):
        for n in self.dsem:
            self._wait(eng, (self.dsem[n], self.dcnt[n], 'dma'))
        for e in ENG:
            if e != eng and self.cnt[e] > 0:
                self._wait(eng, (self.sem[e], self.cnt[e], e))

    def emit(self):
        nc = self.nc
        with nc.Block() as block:
            for e in ENG:
                prog = self.prog[e]
                if not prog:
                    continue

                def body(engine, prog=prog):
                    for f in prog:
                        f(engine)
                getattr(block, BLK[e])(body)


class Arena:
    def __init__(self, nc, nbytes):
        self.t = nc.alloc_sbuf_tensor("arena", [128, nbytes // 2], BF16)
        self.top = 0
        self.cap = nbytes
        self.peak = 0

    def alloc(self, shape_free, dtype):
        n = int(np.prod(shape_free))
        esz = 4 if dtype == F32 else 2
        nb = (n * esz + 31) // 32 * 32
        off = self.top
        self.top += nb
        self.peak = max(self.peak, self.top)
        assert self.top <= self.cap, f"SBUF arena overflow {self.top} > {self.cap}"
        ap = self.t[:, off // 2: off // 2 + n * esz // 2]
        if dtype == F32:
            ap = ap.bitcast(F32)
        if len(shape_free) > 1:
            names = " ".join(f"d{i}" for i in range(len(shape_free)))
            kw = {f"d{i}": int(s) for i, s in enumerate(shape_free)}
            ap = ap.rearrange(f"p ({names}) -> p {names}", **kw)
        return ap

    def view(self, off, shape_free, dtype):
        n = int(np.prod(shape_free))
        esz = 4 if dtype == F32 else 2
        ap = self.t[:, off // 2: off // 2 + n * esz // 2]
        if dtype == F32:
            ap = ap.bitcast(F32)
        if len(shape_free) > 1:
            names = " ".join(f"d{i}" for i in range(len(shape_free)))
            kw = {f"d{i}": int(s) for i, s in enumerate(shape_free)}
            ap = ap.rearrange(f"p ({names}) -> p {names}", **kw)
        return ap

    def mark(self):
        return self.top

    def release(self, m):
        self.top = m


D = 1024
NH = 8
DH = 64
NSLOT = 64
NOWN = 16
NE = 32
EPS = 1e-6
NEG = -1.0e30
N_EXP_RUN = 32
DEBUG = False
import os as _os
A_LEVEL = int(_os.environ.get('A_LEVEL', '9'))
D_LEVEL = int(_os.environ.get('D_LEVEL', '9'))

_PERM = np.concatenate([np.arange(768, 896), np.arange(1024, 1152), np.arange(512, 640), np.arange(640, 768),
                        np.arange(896, 1024), np.arange(1152, 1280), np.arange(0, 512), np.arange(1280, 1304),
                        np.arange(1304, 1816), np.arange(1816, 2328)])
NROWA = 64 + 192 + 512 + 512 + 512 + 128
NROWD = 1024 + 32


def _host_consts(j):
    pad = 3 - j
    f = np.float32
    p = np.arange(128)
    slopes = (2.0 ** (-(np.arange(8) + 1.0))).astype(np.float64)
    dt = np.arange(64)
    biasT = slopes[None, None, :] * (-128.0 * dt[None, :, None] + p[:, None, None] - 127.0)
    ii = np.arange(16)
    ct = np.arange(4)
    cend = 16.0 * (128 * ct[None, None, :, None] + p[:, None, None, None]) + 31.0
    tref = 128.0 * (4 * ii[None, :, None, None] + 3) + 127.0
    biasC = np.minimum(slopes[None, None, None, :] * (cend - tref), 0.0)
    q = np.arange(128)
    cendd = 16 * (128 * (ii[None, :, None] // 4) + p[:, None, None]) + 31
    tq = 128 * (4 * ii[None, :, None] + 3) + q[None, None, :]
    cmaskT = (cendd <= tq).astype(f)
    cidx = 128 * ct[None, :] + p[:, None]
    cvalid = ((cidx >= 8 * pad) & (cidx <= 510)).astype(f)
    n = np.arange(128)
    c0 = 16 * cidx[:, :, None]
    n0 = 64 * n[None, None, :]
    ov = np.clip(np.minimum(c0 + 32, n0 + 64) - np.maximum(c0, n0), 0, None).astype(f) / 32.0
    ovaug = np.concatenate([cvalid[:, :, None], ov * cvalid[:, :, None]], axis=2)
    t = 128 * (4 * ii[:, None, None] + 3) + q[None, :, None]
    nn = n[None, None, :]
    forced = (nn == t // 64) | (nn == 2 * pad)
    invalid = (64 * nn > t) | (nn < 2 * pad)
    addmask = np.where(invalid, NEG, np.where(forced, 1.0e4, 0.0)).astype(f)
    addmask = np.ascontiguousarray(addmask.transpose(1, 0, 2))
    kvalid = np.broadcast_to((np.arange(64) >= pad).astype(f)[None, :], (128, 64)).copy()
    tri = (p[:, None] <= q[None, :]).astype(f)
    triu = (p[:, None] > q[None, :]).astype(f)
    return dict(biasT=biasT.astype(f).reshape(128, 512), biasC=biasC.astype(f).reshape(128, 512),
                cmaskT=cmaskT.reshape(128, 2048), cvalid=cvalid, ovaug=ovaug.reshape(128, 4 * 129),
                addmask=addmask.reshape(128, 2048), kvalid=kvalid,
                trim=np.concatenate([tri, triu], axis=1))


def _host_weights(inp):
    f = np.float32
    w = {}
    w_in = inp["w_in"][:, _PERM]
    w["w_in"] = np.ascontiguousarray(w_in.reshape(8, 128, 2328).transpose(1, 0, 2))
    w["g1"] = np.ascontiguousarray(inp["norm1_g"].reshape(8, 128).T)
    kg = inp["k_norm_g"]
    w["rowA"] = np.concatenate([inp["q_norm_g"], kg[1], kg[2], kg[0], inp["gm_v_norm_g"], inp["out_norm_attn_g"],
                                inp["out_norm_gm_g"], inp["b_cmp2"][0], inp["b_cmp2"][1]]).astype(f)[None, :]
    w["rowD"] = np.concatenate([inp["norm2_g"], inp["b_router"]]).astype(f)[None, :]
    w1 = inp["w_cmp1"].reshape(2, 32, 64, 128).transpose(2, 0, 1, 3)
    w["w1s"] = np.ascontiguousarray(np.concatenate([w1, w1], axis=0)).reshape(128, 2 * 32 * 128)
    pos = inp["cmp_pos"].transpose(2, 0, 1)
    w["posT"] = np.ascontiguousarray(np.concatenate([pos, pos], axis=0)).reshape(128, 64)
    w["b1c"] = np.ascontiguousarray(inp["b_cmp1"].T)
    w["w2"] = np.ascontiguousarray(inp["w_cmp2"].transpose(1, 0, 2)).reshape(128, 128)
    w["wsT"] = np.ascontiguousarray(inp["gm_w_s"].transpose(2, 0, 1)).reshape(128, 1024)
    w["bsT"] = np.ascontiguousarray(inp["gm_b_s"].T)
    w["w_out"] = np.ascontiguousarray(inp["w_out"].reshape(8, 128, 1024).transpose(1, 0, 2)).reshape(128, 8192)
    w["w_router"] = np.ascontiguousarray(inp["w_router"].reshape(8, 128, 32).transpose(1, 0, 2)).reshape(128, 256)
    wgu = inp["w_gate_up"].reshape(NE, 8, 128, 8, 128, 2).transpose(0, 3, 2, 1, 5, 4)
    w["wgu"] = np.ascontiguousarray(wgu).reshape(NE * 8, 128, 8 * 2 * 128)
    w["bgu"] = np.ascontiguousarray(inp["b_gate_up"].reshape(NE, 8, 128, 2).transpose(2, 0, 1, 3)).reshape(128, NE * 16)
    w["wd"] = np.ascontiguousarray(inp["w_down"].reshape(NE, 8, 128, 1024).transpose(0, 2, 1, 3)).reshape(NE, 128, 8192)
    w["b_down"] = np.ascontiguousarray(inp["b_down"])
    return w


def build_program(n_exp=N_EXP_RUN, debug=DEBUG, stop_after=None):
    nc = bass.Bass("TRN2", target_bir_lowering=False)

    def din(name, shape):
        return nc.dram_tensor(name, shape, F32, kind="ExternalInput").ap()

    xT_d = din("xT", [64, 128, 1024])
    xown_d = din("xown", [16, 128, 1024])
    biasT_d = din("biasT", [128, 512])
    biasC_d = din("biasC", [128, 512])
    cmaskT_d = din("cmaskT", [128, 2048])
    cvalid_d = din("cvalid", [128, 4])
    ovaug_d = din("ovaug", [128, 516])
    addmask_d = din("addmask", [128, 2048])
    kvalid_d = din("kvalid", [128, 64])
    trim_d = din("trim", [128, 256])
    w_in_d = din("w_in", [128, 8, 2328])
    g1_d = din("g1", [128, 8])
    rowA_d = din("rowA", [1, NROWA])
    rowD_d = din("rowD", [1, NROWD])
    w1s_d = din("w1s", [128, 8192])
    posT_d = din("posT", [128, 64])
    b1c_d = din("b1c", [128, 2])
    w2_d = din("w2", [128, 128])
    wsT_d = din("wsT", [128, 1024])
    bsT_d = din("bsT", [128, 8])
    w_out_d = din("w_out", [128, 8192])
    w_router_d = din("w_router", [128, 256])
    wgu_d = din("wgu", [max(n_exp, 1) * 8, 128, 2048])
    bgu_d = din("bgu", [128, NE * 16])
    wd_d = din("wd", [max(n_exp, 1), 128, 8192])
    b_down_d = din("b_down", [NE, 1024])
    out_d = nc.dram_tensor("out", [16, 128, 1024], F32, kind="ExternalOutput").ap()
    dbg_outs = {}

    st = ExitStack()
    with st:
        S = Sched(nc, st)
        A = Arena(nc, 191 * 1024)
        ps = [nc.alloc_psum_tensor(f"ps{b}", [128, 512], F32) for b in range(8)]
        P = [p_[:, :] for p_ in ps]
        PB = [p_[:, :].bitcast(BF16) for p_ in ps]

        def _finish():
            S.finish('sp')
            S.emit()
            print("SBUF peak", A.peak, "ops", S.nops, "cnt", S.cnt)
            return nc, dbg_outs

        def dbg(name, ap, n, reads=()):
            if not debug:
                return
            t = nc.dram_tensor("dbg_" + name, [128, n], F32, kind="ExternalOutput").ap()
            dbg_outs[name] = t
            tmp = A.alloc([n], F32)
            S.op('dve', lambda e, o=tmp, i=ap: e.tensor_copy(out=o, in_=i), reads=list(reads), writes=['dbg_' + name])
            S.dma('sp', t, tmp, reads=['dbg_' + name], name='out')
            S.barrier()

        eps_holder = [None]

        def rsqrt_act(out_ap, in_ap, scale, reads, writes):
            S.op('act', lambda e, o=out_ap, i=in_ap, s=scale, b_=eps_holder[0]: e.activation(out=o, in_=i, func=AF.Ln, bias=b_, scale=s),
                 reads=reads, writes=writes)
            S.op('act', lambda e, o=out_ap: e.activation(out=o, in_=o, func=AF.Exp, scale=-0.5),
                 reads=writes, writes=writes)

        identb = A.alloc([128], BF16)
        identf = A.alloc([128], F32)
        onesb = A.alloc([8], BF16)
        epsc = A.alloc([8], F32)
        EPS_AP = epsc[:, 0:1]
        eps_holder[0] = EPS_AP
        rowA = A.alloc([NROWA], F32)
        qg8 = A.alloc([64], F32)
        biasT = A.alloc([64, 8], F32)
        biasC = A.alloc([16, 4, 8], F32)
        cmaskT = A.alloc([16, 128], BF16)
        trim = A.alloc([2, 128], BF16)
        cvalid = A.alloc([4], F32)
        rown = A.alloc([16], F32)
        Kst = A.alloc([2, 8192], BF16)
        Vaug = A.alloc([2, 64, 2, 65], BF16)
        KcT = A.alloc([512], BF16)
        Rc = A.alloc([4, 2, 193], BF16)
        kvt = A.alloc([64], F32)
        base_top = A.mark()

        S.op('pool', lambda e: e.memset(identf, 1.0), writes=['identf'])
        S.op('pool', lambda e: e.affine_select(out=identf, in_=identf, pattern=[[-1, 128]], compare_op=ALU.is_equal,
                                              fill=0.0, base=0, channel_multiplier=1), reads=['identf'], writes=['identf'])
        S.op('dve', lambda e: e.tensor_copy(out=identb, in_=identf), reads=['identf'], writes=['identb'])
        S.op('dve', lambda e: e.memset(onesb, 1.0), writes=['onesb'])
        S.op('dve', lambda e: e.memset(epsc, EPS), writes=['epsc'])
        S.dma('sp', rowA, rowA_d[0:1, :].to_broadcast([128, NROWA]), writes=['c0'], name='c0')
        S.dma('sp', biasT.rearrange("p a b -> p (a b)"), biasT_d, writes=['c0'], name='c0')
        S.dma('sp', biasC.rearrange("p a b c -> p (a b c)"), biasC_d, writes=['c0'], name='c0')
        S.dma('sp', cvalid, cvalid_d, writes=['c0'], name='c0')
        S.dma('pool', cmaskT.rearrange("p a b -> p (a b)"), cmaskT_d, writes=['c0p'], name='c0p')
        S.dma('pool', trim.rearrange("p a b -> p (a b)"), trim_d, writes=['c0p'], name='c0p')
        S.dma('sp', kvt, kvalid_d, writes=['kvt'], name='c0b')
        for kind in range(2):
            for g in range(2):
                S.op('dve', lambda e, kind=kind, g=g: e.tensor_copy(out=Vaug[:, kind, :, g, 64:65], in_=kvt.unsqueeze(2)),
                     reads=['kvt'], writes=['Vcol'])
        ov_v = ovaug_d.rearrange("p (c n) -> p c n", n=129)
        for g in range(2):
            S.dma('pool', Rc[:, :, g, 64:193], ov_v, writes=['c0p'], name='c0p')
        S.op('dve', lambda e: e.tensor_scalar(out=qg8, in0=rowA[:, 0:64], scalar1=0.125, scalar2=None, op0=ALU.mult),
             reads=['c0'], writes=['qg8'])
        if stop_after == 'init':
            S.barrier()
            dbg("rowA", rowA[:, 0:256], 256)
            dbg("Rc0", Rc[:, 0, :, :].rearrange("p g d -> p (g d)"), 386)
            dbg("cmask", cmaskT[:, 3, :], 128)
            dbg("Vcol", Vaug[:, 0, :, 0, 64:65].rearrange("p a b -> p (a b)"), 64)
            return _finish()
        kg12 = rowA[:, 64:192].rearrange("p (k d) -> p k d", d=64)
        kg0 = rowA[:, 192:256]
        gmv = rowA[:, 256:768]
        ona = rowA[:, 768:1280]
        ong = rowA[:, 1280:1792]
        b2k = rowA[:, 1792:1856]
        b2v = rowA[:, 1856:1920]

        KCraw = A.alloc([2, 8224], BF16)
        Wkv = A.alloc([8, 768], BF16)
        g1c = A.alloc([8], F32)
        w1s = A.alloc([2, 32, 128], BF16)
        posTb = A.alloc([2, 32], BF16)
        b1c = A.alloc([2], F32)
        w2b = A.alloc([2, 64], BF16)
        xTb = [A.alloc([8, 128], BF16) for _ in range(2)]
        sqb = [A.alloc([8, 128], BF16) for _ in range(2)]
        z1 = [A.alloc([256], F32) for _ in range(2)]
        zt = [A.alloc([256], F32) for _ in range(2)]
        ktok = [A.alloc([512], BF16) for _ in range(2)]
        rr = [A.alloc([8], F32) for _ in range(2)]
        Wkv32 = [A.alloc([768], F32) for _ in range(2)]

        S.dma('sp', g1c, g1_d, writes=['g1c'], name='c1')
        S.dma('sp', b1c, b1c_d, writes=['b1c'], name='c1')
        S.dma('pool', w1s.rearrange("p a b c -> p (a b c)"), w1s_d, writes=['w1s'], name='c1p')
        S.dma('pool', posTb.rearrange("p a b -> p (a b)"), posT_d, writes=['posTb'], name='c1p')
        S.dma('pool', w2b.rearrange("p a b -> p (a b)"), w2_d, writes=['w2b'], name='c1p')
        for kc in range(8):
            wb = Wkv32[kc % 2]
            S.dma('sp', wb, w_in_d[:, kc, 0:768], writes=[f'Wkv32{kc%2}'], name=f'Wkv32{kc%2}')
            S.op('dve', lambda e, kc=kc, wb=wb: e.tensor_scalar(out=Wkv[:, kc, :], in0=wb, scalar1=g1c[:, kc:kc + 1],
                                                                scalar2=None, op0=ALU.mult),
                 reads=[f'Wkv32{kc%2}', 'g1c'], writes=['Wkv'])
        S.op('pool', lambda e: e.memset(KCraw[:, :, 8192:8224], 0.0), writes=['KCpad'])
        if NSLOT < 64:
            S.op('pool', lambda e: e.memset(KCraw[:, :, NSLOT * 128:8192], 0.0), writes=['KCpad'])

        for s in range(NSLOT):
            b = s % 2
            xb, sq, z1b, ztb, kt_, rb = xTb[b], sqb[b], z1[b], zt[b], ktok[b], rr[b]
            pA, pB, pT = P[3 * b], P[3 * b + 1], PB[3 * b + 2]
            kx, ksq, kz, kzt, kk, kr = f'xTb{b}', f'sq{b}', f'z1{b}', f'zt{b}', f'ktok{b}', f'rr{b}'
            kpA, kpB, kpT = f'ps{3*b}', f'ps{3*b+1}', f'ps{3*b+2}'
            S.dma('pool', xb.rearrange("p a b -> p (a b)"), xT_d[s], writes=[kx], name=kx)
            S.op('act', lambda e, o=sq, i=xb: e.activation(out=o, in_=i, func=AF.Square), reads=[kx], writes=[ksq])
            for kc in range(8):
                S.op('pe', lambda e, kc=kc, o=pB, l=sq: e.matmul(o[:, 256:257], lhsT=l[:, kc, :], rhs=onesb[:, 0:1],
                                                                start=(kc == 0), stop=(kc == 7)),
                     reads=[ksq, 'onesb'], writes=[kpB], signal=False)
            for kc in range(8):
                S.op('pe', lambda e, kc=kc, o=pA, l=xb: e.matmul(o[:, 0:512], lhsT=l[:, kc, :], rhs=Wkv[:, kc, 0:512],
                                                                start=(kc == 0), stop=(kc == 7)),
                     reads=[kx, 'Wkv'], writes=[kpA], signal=False)
            for kc in range(8):
                S.op('pe', lambda e, kc=kc, o=pB, l=xb: e.matmul(o[:, 0:256], lhsT=l[:, kc, :], rhs=Wkv[:, kc, 512:768],
                                                                start=(kc == 0), stop=(kc == 7)),
                     reads=[kx, 'Wkv'], writes=[kpB], signal=(kc == 7))
            if A_LEVEL < 1:
                continue
            rsqrt_act(rb[:, 0:1], pB[:, 256:257], 1.0 / 1024.0, [kpB, 'epsc'], [kr])
            rcol = rb[:, 0:1]
            if A_LEVEL < 2:
                continue
            S.op('act', lambda e, o=z1b, i=pA, r_=rcol: e.mul(out=o, in_=i[:, 0:256], mul=r_), reads=[kpA, kr], writes=[kz])
            S.op('act', lambda e, o=kt_, i=pA, r_=rcol: e.mul(out=o[:, 256:512], in_=i[:, 256:512], mul=r_),
                 reads=[kpA, kr], writes=[kk])
            S.op('act', lambda e, s=s, i=pB, r_=rcol: e.mul(out=Vaug[:, :, s, :, 0:64],
                                                          in_=i[:, 0:256].rearrange("p (k g d) -> p k g d", k=2, g=2), mul=r_),
                 reads=[kpB, kr], writes=[('V', s)])
            if s % 4 == 3:
                S.op('act', lambda e, s=s, r_=rcol: e.copy(out=rown[:, s // 4:s // 4 + 1], in_=r_), reads=[kr], writes=['rown'])
            if A_LEVEL < 3:
                continue
            S.op('dve', lambda e, o=ztb, i=z1b: e.tensor_tensor(out=o, in0=i, in1=i, op=ALU.mult), reads=[kz], writes=[kzt])
            S.op('dve', lambda e, o=rb, i=ztb: e.tensor_reduce(out=o[:, 4:8], in_=i.rearrange("p (a d) -> p a d", d=64),
                                                             axis=AX.X, op=ALU.add), reads=[kzt], writes=[kr + 'k'])
            rsqrt_act(rb[:, 4:8], rb[:, 4:8], 1.0 / 64.0, [kr + 'k', 'epsc'], [kr + 'k'])
            S.op('dve', lambda e, o=ztb, i=z1b, r_=rb: e.tensor_tensor(
                out=o.rearrange("p (a d) -> p a d", d=64), in0=i.rearrange("p (a d) -> p a d", d=64),
                in1=r_[:, 4:8].unsqueeze(2).to_broadcast([128, 4, 64]), op=ALU.mult), reads=[kz, kr + 'k'], writes=[kzt])
            S.op('dve', lambda e, o=kt_, i=ztb: e.tensor_tensor(
                out=o[:, 0:256].rearrange("p (k g d) -> p k g d", k=2, g=2),
                in0=i.rearrange("p (k g d) -> p k g d", k=2, g=2),
                in1=kg12.unsqueeze(2).to_broadcast([128, 2, 2, 64]), op=ALU.mult), reads=[kzt, 'c0'], writes=[kk])
            if A_LEVEL < 4:
                continue
            for t4 in range(4):
                S.op('pe', lambda e, t4=t4, o=pT, i=kt_: e.transpose(o[:, t4 * 128:(t4 + 1) * 128], i[:, t4 * 128:(t4 + 1) * 128], identb),
                     reads=[kk, 'identb'], writes=[kpT], signal=(t4 == 3))
            if A_LEVEL < 5:
                continue
            S.op('dve', lambda e, s=s, i=pT: e.tensor_copy(out=Kst[:, :, s * 128:(s + 1) * 128],
                                                         in_=i[:, 0:256].rearrange("p (a t) -> p a t", t=128)),
                 reads=[kpT], writes=[('K', s)])
            if A_LEVEL < 6:
                continue
            S.op('dve', lambda e, s=s, i=pT: e.tensor_copy(out=KCraw[:, :, s * 128:(s + 1) * 128],
                                                  in_=i[:, 256:512].rearrange("p (a t) -> p a t", t=128)),
                 reads=[kpT], writes=[('KC', s)])
        S.barrier()
        if debug:
            dbg("Ksel0", Kst[:, 0, 3 * 128:4 * 128], 128)
            dbg("Kwin0", Kst[:, 1, 3 * 128:4 * 128], 128)
            dbg("Vsel0", Vaug[:, 0, 3, :, :].rearrange("p g d -> p (g d)"), 130)
            dbg("KCraw0", KCraw[:, 0, 3 * 128:4 * 128], 128)
        if stop_after == 'A':
            return _finish()

        chid = A.alloc([2], F32)
        gT = A.alloc([512], BF16)
        kc32 = A.alloc([4, 64], F32)
        kcsq = A.alloc([4, 64], F32)
        kcn = A.alloc([4, 2, 64], BF16)
        rcs = A.alloc([4], F32)
        for kind in range(2):
            for l in range(32):
                S.op('pe', lambda e, kind=kind, l=l: e.matmul(P[7][:, kind:kind + 1], lhsT=w1s[0:64, kind, l, :],
                                                              rhs=posTb[0:64, kind, l:l + 1], start=(l == 0), stop=(l == 31)),
                     reads=['w1s', 'posTb'], writes=['ps7'], signal=(l == 31))
        S.op('dve', lambda e: e.tensor_tensor(out=chid, in0=P[7][:, 0:2], in1=b1c, op=ALU.add), reads=['ps7', 'b1c'], writes=['chid'])
        for kind in range(2):
            for g in range(2):
                hp = P[(kind * 2 + g) % 2]
                khp = f'ps{(kind * 2 + g) % 2}'
                for l in range(32):
                    base = 0 if l < 16 else 16
                    rhs = KCraw[g * 64:(g + 1) * 64, kind, base:base + 8192].rearrange("p (c s) -> p s c", s=16)[:, l - base, :]
                    S.op('pe', lambda e, kind=kind, g=g, l=l, rhs=rhs, hp=hp: e.matmul(
                        hp, lhsT=w1s[g * 64:(g + 1) * 64, kind, l, :], rhs=rhs, start=(l == 0), stop=(l == 31)),
                         reads=['w1s'], writes=[khp], signal=(l == 31))
                S.op('act', lambda e, kind=kind, hp=hp: e.activation(out=gT, in_=hp, func=AF.Gelu_apprx_tanh, bias=chid[:, kind:kind + 1]),
                     reads=[khp, 'chid'], writes=['gT'])
                cp = P[2 + (kind * 2 + g) % 2]
                kcp = f'ps{2 + (kind * 2 + g) % 2}'
                for ct in range(4):
                    S.op('pe', lambda e, kind=kind, ct=ct, cp=cp: e.matmul(cp[:, ct * 64:(ct + 1) * 64], lhsT=gT[:, ct * 128:(ct + 1) * 128],
                                                                         rhs=w2b[:, kind, :], start=True, stop=True),
                         reads=['gT', 'w2b'], writes=[kcp], signal=(ct == 3))
                cpv = cp[:, 0:256].rearrange("p (c d) -> p c d", d=64)
                if kind == 0:
                    S.op('dve', lambda e, cpv=cpv: e.tensor_tensor(out=kc32, in0=cpv, in1=b2k.unsqueeze(1).to_broadcast([128, 4, 64]), op=ALU.add),
                         reads=[kcp, 'c0'], writes=['kc32'])
                    S.op('dve', lambda e: e.tensor_tensor(out=kcsq, in0=kc32, in1=kc32, op=ALU.mult), reads=['kc32'], writes=['kcsq'])
                    S.op('dve', lambda e: e.tensor_reduce(out=rcs, in_=kcsq, axis=AX.X, op=ALU.add), reads=['kcsq'], writes=['rcs'])
                    rsqrt_act(rcs, rcs, 1.0 / 64.0, ['rcs', 'epsc'], ['rcs'])
                    S.op('dve', lambda e: e.tensor_tensor(out=kcsq, in0=kc32, in1=rcs.unsqueeze(2).to_broadcast([128, 4, 64]), op=ALU.mult),
                         reads=['kc32', 'rcs'], writes=['kcsq'])
                    S.op('dve', lambda e, g=g: e.tensor_tensor(out=kcn[:, :, g, :], in0=kcsq, in1=kg0.unsqueeze(1).to_broadcast([128, 4, 64]), op=ALU.mult),
                         reads=['kcsq', 'c0'], writes=['kcn'])
                else:
                    S.op('dve', lambda e, cpv=cpv: e.tensor_tensor(out=kc32, in0=cpv, in1=b2v.unsqueeze(1).to_broadcast([128, 4, 64]), op=ALU.add),
                         reads=[kcp, 'c0'], writes=['kc32'])
                    S.op('dve', lambda e, g=g: e.tensor_tensor(out=Rc[:, :, g, 0:64], in0=kc32, in1=cvalid.unsqueeze(2).to_broadcast([128, 4, 64]), op=ALU.mult),
                         reads=['kc32', 'c0'], writes=['Rc'])
            if kind == 0:
                for ct in range(4):
                    S.op('pe', lambda e, ct=ct: e.transpose(PB[4][:, ct * 128:(ct + 1) * 128], kcn[:, ct, :, :].rearrange("p g d -> p (g d)"), identb),
                         reads=['kcn', 'identb'], writes=['ps4'], signal=(ct == 3))
                S.op('dve', lambda e: e.tensor_copy(out=KcT, in_=PB[4][:, 0:512]), reads=['ps4'], writes=['KcT'])
        S.barrier()
        if debug:
            dbg("KcT", KcT, 512)
            dbg("Rc0", Rc[:, 0, :, :].rearrange("p g d -> p (g d)"), 386)
        if stop_after == 'B':
            return _finish()
        A.release(base_top)

        mixedT = A.alloc([8, 2048], BF16)
        QT = A.alloc([16, 4, 128], BF16)
        gsig = A.alloc([16, 24], F32)
        mark2 = A.mark()
        Wr = A.alloc([8, 1560], BF16)
        g1c2 = A.alloc([8], F32)
        wsTm = A.alloc([8, 128], BF16)
        bsT = A.alloc([8], F32)
        xb2 = [A.alloc([8, 128], BF16) for _ in range(2)]
        zq = A.alloc([512], F32)
        zq2 = A.alloc([512], F32)
        qn = A.alloc([512], BF16)
        u32 = A.alloc([512], F32)
        v32 = A.alloc([512], F32)
        vn = A.alloc([512], BF16)
        og_off = A.mark()
        og = A.alloc([512], F32)
        og2 = A.alloc([512], F32)
        wsT32 = A.view(og_off, [8, 128], F32)
        mg = A.alloc([512], BF16)
        st8 = A.alloc([16], F32)
        Wr32 = [A.alloc([1560], F32)]

        S.dma('sp', g1c2, g1_d, writes=['g1c2'], name='c2')
        S.dma('sp', wsT32.rearrange("p a b -> p (a b)"), wsT_d, writes=['wsT32'], name='c2')
        S.dma('sp', bsT, bsT_d, writes=['bsT'], name='c2')
        S.op('dve', lambda e: e.tensor_tensor(out=wsTm, in0=wsT32, in1=trim[:, 0, :].unsqueeze(1).to_broadcast([128, 8, 128]), op=ALU.mult),
             reads=['wsT32'], writes=['wsTm'])
        for kc in range(8):
            wb = Wr32[0]
            S.dma('sp', wb, w_in_d[:, kc, 768:2328], writes=['Wr320'], name='Wr320')
            S.op('dve', lambda e, kc=kc, wb=wb: e.tensor_scalar(out=Wr[:, kc, :], in0=wb, scalar1=g1c2[:, kc:kc + 1], scalar2=None, op0=ALU.mult),
                 reads=['Wr320', 'g1c2'], writes=['Wr'])

        for i in range(NOWN):
            xb = xb2[i % 2]
            kx = f'xb2{i%2}'
            S.dma('pool', xb.rearrange("p a b -> p (a b)"), xT_d[4 * i + 3], writes=[kx], name=kx)
            specs = [(0, 0, 512, 0), (1, 0, 24, 512), (2, 0, 512, 536), (3, 0, 512, 1048)]
            for (bk, o0, n, c0) in specs:
                for kc in range(8):
                    S.op('pe', lambda e, bk=bk, n=n, c0=c0, kc=kc, xb=xb: e.matmul(P[bk][:, 0:n], lhsT=xb[:, kc, :], rhs=Wr[:, kc, c0:c0 + n],
                                                                            start=(kc == 0), stop=(kc == 7)),
                         reads=[kx, 'Wr'], writes=[f'ps{bk}'], signal=(kc == 7))
            rcol = rown[:, i:i + 1]
            S.op('act', lambda e, r_=rcol: e.mul(out=zq, in_=P[0], mul=r_), reads=['ps0'], writes=['zq'])
            S.op('act', lambda e, i=i, r_=rcol: e.activation(out=gsig[:, i, :], in_=P[1][:, 0:24], func=AF.Sigmoid, scale=r_),
                 reads=['ps1'], writes=['gsig'])
            S.op('act', lambda e, r_=rcol: e.activation(out=u32, in_=P[2], func=AF.Gelu_apprx_tanh, scale=r_), reads=['ps2'], writes=['u32'])
            S.op('act', lambda e, r_=rcol: e.activation(out=v32, in_=P[3], func=AF.Gelu_apprx_tanh, scale=r_), reads=['ps3'], writes=['v32'])
            S.op('dve', lambda e: e.tensor_tensor(out=zq2, in0=zq, in1=zq, op=ALU.mult), reads=['zq'], writes=['zq2'])
            S.op('dve', lambda e: e.tensor_reduce(out=st8[:, 0:8], in_=zq2.rearrange("p (h d) -> p h d", d=64), axis=AX.X, op=ALU.add),
                 reads=['zq2'], writes=['ssq'])
            rsqrt_act(st8[:, 0:8], st8[:, 0:8], 1.0 / 64.0, ['ssq'], ['ssq'])
            S.op('dve', lambda e: e.tensor_tensor(out=zq2.rearrange("p (h d) -> p h d", d=64), in0=zq.rearrange("p (h d) -> p h d", d=64),
                                                  in1=st8[:, 0:8].unsqueeze(2).to_broadcast([128, 8, 64]), op=ALU.mult),
                 reads=['zq', 'ssq'], writes=['zq2'])
            S.op('dve', lambda e: e.tensor_tensor(out=qn.rearrange("p (r g d) -> p g r d", r=4, g=2),
                                                  in0=zq2.rearrange("p (g r d) -> p g r d", g=2, r=4),
                                                  in1=qg8.unsqueeze(1).unsqueeze(1).to_broadcast([128, 2, 4, 64]), op=ALU.mult),
                 reads=['zq2', 'qg8'], writes=['qn'])
            for r4 in range(4):
                S.op('pe', lambda e, r4=r4: e.transpose(PB[4][:, r4 * 128:(r4 + 1) * 128], qn[:, r4 * 128:(r4 + 1) * 128], identb),
                     reads=['qn'], writes=['ps4'], signal=(r4 == 3))
            S.op('dve', lambda e, i=i: e.tensor_copy(out=QT[:, i, :, :].rearrange("p r q -> p (r q)"), in_=PB[4][:, 0:512]),
                 reads=['ps4'], writes=['QT'])
            S.op('dve', lambda e: e.tensor_tensor(out=og, in0=v32, in1=v32, op=ALU.mult), reads=['v32'], writes=['og'])
            S.op('dve', lambda e: e.tensor_reduce(out=st8[:, 8:9], in_=og, axis=AX.X, op=ALU.add), reads=['og'], writes=['ssv'])
            rsqrt_act(st8[:, 8:9], st8[:, 8:9], 1.0 / 512.0, ['ssv'], ['ssv'])
            S.op('dve', lambda e: e.scalar_tensor_tensor(out=vn, in0=v32, scalar=st8[:, 8:9], in1=gmv, op0=ALU.mult, op1=ALU.mult),
                 reads=['v32', 'ssv'], writes=['vn'])
            for g8 in range(8):
                S.op('pe', lambda e, g8=g8: e.matmul(P[5][:, g8 * 64:(g8 + 1) * 64], lhsT=wsTm[:, g8, :], rhs=vn[:, g8 * 64:(g8 + 1) * 64],
                                                     start=True, stop=True),
                     reads=['vn', 'wsTm'], writes=['ps5'], signal=(g8 == 7))
            S.op('dve', lambda e: e.tensor_tensor(out=og.rearrange("p (g d) -> p g d", d=64), in0=P[5].rearrange("p (g d) -> p g d", d=64),
                                                  in1=bsT.unsqueeze(2).to_broadcast([128, 8, 64]), op=ALU.add),
                 reads=['ps5', 'bsT'], writes=['og'])
            S.op('dve', lambda e: e.tensor_tensor(out=og, in0=og, in1=u32, op=ALU.mult), reads=['og', 'u32'], writes=['og'])
            S.op('dve', lambda e: e.tensor_tensor(out=og2, in0=og, in1=og, op=ALU.mult), reads=['og'], writes=['og2'])
            S.op('dve', lambda e: e.tensor_reduce(out=st8[:, 9:10], in_=og2, axis=AX.X, op=ALU.add), reads=['og2'], writes=['sso'])
            rsqrt_act(st8[:, 9:10], st8[:, 9:10], 1.0 / 512.0, ['sso'], ['sso'])
            S.op('dve', lambda e: e.scalar_tensor_tensor(out=mg, in0=og, scalar=st8[:, 9:10], in1=ong, op0=ALU.mult, op1=ALU.mult),
                 reads=['og', 'sso'], writes=['mg'])
            for c in range(4):
                S.op('pe', lambda e, c=c: e.transpose(PB[6][:, c * 128:(c + 1) * 128], mg[:, c * 128:(c + 1) * 128], identb),
                     reads=['mg'], writes=['ps6'], signal=(c == 3))
            S.op('dve', lambda e, i=i: e.tensor_copy(out=mixedT[:, 4:8, i * 128:(i + 1) * 128], in_=PB[6][:, 0:512].rearrange("p (c t) -> p c t", t=128)),
                 reads=['ps6'], writes=['mixedT'])
        S.barrier()
        if debug:
            dbg("QT0", QT[:, 0, 0, :], 128)
            dbg("gsig", gsig.rearrange("p a b -> p (a b)"), 384)
            dbg("mgm0", mixedT[:, 4, 0:128], 128)
        if stop_after == 'A2':
            return _finish()
        A.release(mark2)

        am = [A.alloc([128], F32) for _ in range(2)]
        eT = [A.alloc([4, 128], BF16) for _ in range(3)]
        Pm = [A.alloc([4, 128], BF16) for _ in range(3)]
        oacc = A.alloc([8, 64], F32)
        otmp = A.alloc([4, 64], F32)
        osq = A.alloc([512], F32)
        imp = A.alloc([128], F32)
        imp2 = A.alloc([128], F32)
        m8 = A.alloc([16], F32)
        thr = A.alloc([2], F32)
        selb = [A.alloc([128], BF16) for _ in range(2)]
        selX = A.alloc([128, 64], BF16)
        rz = A.alloc([4], F32)
        gz = A.alloc([4], F32)
        ssa = A.alloc([2], F32)
        ma = A.alloc([512], BF16)
        gs4 = gsig.rearrange("p i (h k) -> p i h k", k=3)

        ring = [0]

        def score_tile(lhsT, rhs, bias_fn, g):
            n = ring[0]
            ring[0] += 1
            sp_, ksp = P[n % 2], f'ps{n % 2}'
            e_, ke = eT[n % 3], f'eT{n % 3}'
            S.op('pe', lambda e, sp_=sp_, lhsT=lhsT, rhs=rhs: e.matmul(sp_, lhsT=lhsT, rhs=rhs, start=True, stop=True),
                 reads=[], writes=[ksp])
            for r in range(4):
                S.op('act', lambda e, r=r, sp_=sp_, e_=e_, b_=bias_fn(4 * g + r): e.activation(
                    out=e_[:, r, :], in_=sp_[:, r * 128:(r + 1) * 128], func=AF.Exp, bias=b_),
                     reads=[ksp], writes=[ke])
            return n, e_, ke

        def qblock(i):
            sq_ = 4 * i + 3
            amt, kam = am[i % 2], f'am{i%2}'
            S.dma('sp', amt, addmask_d[:, i * 128:(i + 1) * 128], writes=[kam], name=kam)
            def grp(g):
                QTg = QT[g * 64:(g + 1) * 64, i, :, :].rearrange("p r q -> p (r q)")
                nct = i // 4 + 1
                U5 = P[5].rearrange("p (r c) -> p r c", c=256)
                U6 = P[6].rearrange("p (r c) -> p r c", c=256)
                Ur = [U5[:, 0, :], U5[:, 1, :], U6[:, 0, :], U6[:, 1, :]]
                for ct in range(nct):
                    n, e_, ke = score_tile(KcT[g * 64:(g + 1) * 64, ct * 128:(ct + 1) * 128], QTg,
                                           lambda h, ct=ct: biasC[:, i, ct, h:h + 1], g)
                    if ct == nct - 1:
                        S.op('pool', lambda e, e_=e_: e.tensor_tensor(out=e_, in0=e_, in1=cmaskT[:, i, :].unsqueeze(1).to_broadcast([128, 4, 128]),
                                                                     op=ALU.mult), reads=[ke], writes=[ke])
                    for r in range(4):
                        S.op('pe', lambda e, r=r, ct=ct, e_=e_: e.matmul(Ur[r][:, 0:193], lhsT=e_[:, r, :], rhs=Rc[:, ct, g, :],
                                                                       start=(ct == 0 and r in (0, 2)), stop=(ct == nct - 1 and r in (1, 3))),
                             reads=[ke], writes=['ps5' if r < 2 else 'ps6'], signal=(r == 3))
                S.op('dve', lambda e: e.tensor_scalar(out=rz[:, 0:2].unsqueeze(2), in0=U5[:, :, 64:65], scalar1=1e-30, scalar2=None, op0=ALU.max),
                     reads=['ps5'], writes=['rz'])
                S.op('dve', lambda e: e.tensor_scalar(out=rz[:, 2:4].unsqueeze(2), in0=U6[:, :, 64:65], scalar1=1e-30, scalar2=None, op0=ALU.max),
                     reads=['ps6'], writes=['rz'])
                S.op('dve', lambda e: e.reciprocal(out=rz, in_=rz), reads=['rz'], writes=['rz'])
                S.op('dve', lambda e, g=g: e.tensor_tensor(out=gz, in0=rz, in1=gs4[:, i, 4 * g:4 * g + 4, 0], op=ALU.mult), reads=['rz'], writes=['gz'])
                S.op('dve', lambda e, g=g: e.tensor_tensor(out=oacc[:, 4 * g:4 * g + 2, :], in0=U5[:, :, 0:64],
                                                           in1=gz[:, 0:2].unsqueeze(2).to_broadcast([128, 2, 64]), op=ALU.mult),
                     reads=['ps5', 'gz'], writes=['oacc'])
                S.op('dve', lambda e, g=g: e.tensor_tensor(out=oacc[:, 4 * g + 2:4 * g + 4, :], in0=U6[:, :, 0:64],
                                                           in1=gz[:, 2:4].unsqueeze(2).to_broadcast([128, 2, 64]), op=ALU.mult),
                     reads=['ps6', 'gz'], writes=['oacc'])
                for r in range(4):
                    S.op('dve', lambda e, r=r: e.scalar_tensor_tensor(out=imp, in0=Ur[r][:, 65:193], scalar=rz[:, r:r + 1],
                                                                      in1=(amt if r == 0 else imp), op0=ALU.mult, op1=ALU.add),
                         reads=['ps5' if r < 2 else 'ps6', 'rz', kam, 'imp'], writes=['imp'])
                S.op('dve', lambda e: e.max(out=m8[:, 0:8], in_=imp), reads=['imp'], writes=['m8a'])
                S.op('dve', lambda e: e.match_replace(out=imp2, in_to_replace=m8[:, 0:8], in_values=imp, imm_value=-3.0e38),
                     reads=['imp', 'm8a'], writes=['imp2'])
                S.op('dve', lambda e: e.max(out=m8[:, 8:16], in_=imp2), reads=['imp2'], writes=['m8b'])
                S.op('dve', lambda e: e.tensor_scalar(out=thr[:, 0:1], in0=m8[:, 15:16], scalar1=-1.0e29, scalar2=None, op0=ALU.max),
                     reads=['m8b'], writes=['thr'])
                sel_, ksel = selb[g], f'sel{g}'
                S.op('dve', lambda e, sel_=sel_: e.tensor_scalar(out=sel_, in0=imp, scalar1=thr[:, 0:1], scalar2=None, op0=ALU.is_ge),
                     reads=['imp', 'thr'], writes=[ksel])
                nb = 2 * (sq_ + 1)
                S.op('pool', lambda e, sel_=sel_: e.tensor_copy(out=selX[:, 0:nb, :], in_=sel_[:, 0:nb].unsqueeze(2).to_broadcast([128, nb, 64])),
                     reads=[ksel], writes=['selX'])
                for br in (1, 2):
                    ob, kob = (P[7], 'ps7') if br == 1 else (P[4], 'ps4')
                    O4 = ob.rearrange("p (r c) -> p r c", c=128)
                    kts = list(range(0, sq_ + 1)) if br == 1 else list(range(max(0, sq_ - 4), sq_ + 1))
                    for kt in kts:
                        n, e_, ke = score_tile(Kst[g * 64:(g + 1) * 64, br - 1, kt * 128:(kt + 1) * 128], QTg,
                                               lambda h, kt=kt: biasT[:, sq_ - kt, h:h + 1], g)
                        src, ksrc = e_, ke
                        if br == 1:
                            pmv = PB[2][:, (n % 4) * 128:(n % 4 + 1) * 128]
                            kpm = f'pm{n % 4}'
                            S.op('pe', lambda e, kt=kt, pmv=pmv: e.transpose(
                                pmv, selX[:, 2 * kt:2 * kt + 2, :].rearrange("p a b -> p (a b)"), identb),
                                 reads=['selX'], writes=[kpm])
                            p_, kp = Pm[n % 3], f'Pm{n % 3}'
                            S.op('dve', lambda e, p_=p_, e_=e_, pmv=pmv: e.tensor_tensor(
                                out=p_, in0=e_, in1=pmv.unsqueeze(1).to_broadcast([128, 4, 128]), op=ALU.mult),
                                 reads=[ke, kpm], writes=[kp])
                            src, ksrc = p_, kp
                            if kt == sq_:
                                S.op('pool', lambda e, p_=p_: e.tensor_tensor(out=p_, in0=p_, in1=trim[:, 0, :].unsqueeze(1).to_broadcast([128, 4, 128]),
                                                                             op=ALU.mult), reads=[kp], writes=[kp])
                        else:
                            if kt == sq_ or kt == sq_ - 4:
                                mi = 0 if kt == sq_ else 1
                                S.op('pool', lambda e, e_=e_, mi=mi: e.tensor_tensor(out=e_, in0=e_, in1=trim[:, mi, :].unsqueeze(1).to_broadcast([128, 4, 128]),
                                                                                   op=ALU.mult), reads=[ke], writes=[ke])
                        for r in range(4):
                            S.op('pe', lambda e, r=r, kt=kt, src=src, O4=O4, br=br, first=(kt == kts[0] and r == 0), last=(kt == kts[-1] and r == 3): e.matmul(
                                O4[:, r, 0:65], lhsT=src[:, r, :], rhs=Vaug[:, br - 1, kt, g, :], start=first, stop=last),
                                 reads=[ksrc], writes=[kob], signal=(r == 3))
                    S.op('dve', lambda e, O4=O4: e.tensor_scalar(out=rz.unsqueeze(2), in0=O4[:, :, 64:65], scalar1=1e-30, scalar2=None, op0=ALU.max),
                         reads=[kob], writes=['rz'])
                    S.op('dve', lambda e: e.reciprocal(out=rz, in_=rz), reads=['rz'], writes=['rz'])
                    S.op('dve', lambda e, g=g, br=br: e.tensor_tensor(out=gz, in0=rz, in1=gs4[:, i, 4 * g:4 * g + 4, br], op=ALU.mult),
                         reads=['rz'], writes=['gz'])
                    S.op('dve', lambda e, O4=O4: e.tensor_tensor(out=otmp, in0=O4[:, :, 0:64], in1=gz.unsqueeze(2).to_broadcast([128, 4, 64]), op=ALU.mult),
                         reads=[kob, 'gz'], writes=['otmp'])
                    S.op('pool', lambda e, g=g: e.tensor_tensor(out=oacc[:, 4 * g:4 * g + 4, :], in0=oacc[:, 4 * g:4 * g + 4, :], in1=otmp, op=ALU.add),
                         reads=['otmp', 'oacc'], writes=['oacc'])
            grp(0)
            grp(1)
            oflat = oacc.rearrange("p h d -> p (h d)")
            if debug and i in (0, 5):
                dbg(f"oattn{i}", oflat, 512, reads=['oacc'])
            S.op('dve', lambda e: e.tensor_tensor(out=osq, in0=oflat, in1=oflat, op=ALU.mult), reads=['oacc'], writes=['osq'])
            S.op('dve', lambda e: e.tensor_reduce(out=ssa[:, 0:1], in_=osq, axis=AX.X, op=ALU.add), reads=['osq'], writes=['ssa'])
            rsqrt_act(ssa[:, 0:1], ssa[:, 0:1], 1.0 / 512.0, ['ssa'], ['ssa'])
            S.op('dve', lambda e: e.scalar_tensor_tensor(out=ma, in0=oflat, scalar=ssa[:, 0:1], in1=ona, op0=ALU.mult, op1=ALU.mult),
                 reads=['oacc', 'ssa'], writes=['ma'])
            for c in range(4):
                S.op('pe', lambda e, c=c: e.transpose(PB[3][:, c * 128:(c + 1) * 128], ma[:, c * 128:(c + 1) * 128], identb),
                     reads=['ma'], writes=['ps3'], signal=(c == 3))
            S.op('dve', lambda e, i=i: e.tensor_copy(out=mixedT[:, 0:4, i * 128:(i + 1) * 128], in_=PB[3][:, 0:512].rearrange("p (c t) -> p c t", t=128)),
                 reads=['ps3'], writes=['mixedT'])
        for i in range(NOWN):
            qblock(i)
        S.barrier()
        if stop_after == 'C':
            return _finish()

        mixed_off = base_top
        assert mixed_off == 88000, mixed_off
        A.top = 0
        acc = A.alloc([16, 1024], F32)
        gmoe = A.alloc([16, 32], F32)
        bgu = A.alloc([NE * 16], F32)
        dA = A.mark()
        identf2 = A.alloc([128], F32)
        identb2 = A.alloc([128], BF16)
        epsD = A.alloc([8], F32)
        eps_holder[0] = epsD[:, 0:1]
        rowD = A.alloc([NROWD], F32)
        wr32 = A.alloc([8, 32], F32)
        wrh = A.alloc([8, 32], BF16)
        wrl = A.alloc([8, 32], BF16)
        std = A.alloc([64], F32)
        lgt = A.alloc([32], F32)
        msk = A.alloc([32], F32)
        ex = A.alloc([32], F32)
        assert A.top <= mixed_off, A.top
        A.top = mixed_off + 32768
        xnT = A.alloc([8, 2048], BF16)
        dB = A.mark()
        wout = A.alloc([8, 1024], BF16)
        xo = [A.alloc([1024], F32) for _ in range(2)]
        xn = A.alloc([1024], F32)
        xh = A.alloc([1024], BF16)
        xl = A.alloc([1024], BF16)
        loT = A.alloc([8, 128], BF16)
        g2bc = rowD[:, 0:1024]
        brow = rowD[:, 1024:1056]

        S.op('pool', lambda e: e.memset(identf2, 1.0), writes=['identf2'])
        S.op('pool', lambda e: e.affine_select(out=identf2, in_=identf2, pattern=[[-1, 128]], compare_op=ALU.is_equal,
                                              fill=0.0, base=0, channel_multiplier=1), reads=['identf2'], writes=['identf2'])
        S.op('dve', lambda e: e.tensor_copy(out=identb2, in_=identf2), reads=['identf2'], writes=['identb2'])
        S.op('dve', lambda e: e.memset(epsD, EPS), writes=['epsD'])
        S.dma('sp', rowD, rowD_d[0:1, :].to_broadcast([128, NROWD]), writes=['d0'], name='d0')
        S.dma('sp', bgu, bgu_d, writes=['d0'], name='d0')
        S.dma('sp', wr32.rearrange("p a b -> p (a b)"), w_router_d, writes=['d0'], name='d0')
        S.dma('pool', wout.rearrange("p a b -> p (a b)"), w_out_d, writes=['d0p'], name='d0p')
        S.op('dve', lambda e: e.tensor_copy(out=wrh, in_=wr32), reads=['d0'], writes=['wrh'])
        S.op('dve', lambda e: e.tensor_tensor(out=wrl, in0=wr32, in1=wrh, op=ALU.subtract), reads=['d0', 'wrh'], writes=['wrl'])

        def dpre(i):
            xot, kxo = xo[i % 2], f'xo{i%2}'
            S.dma('sp', xot, xown_d[i], writes=[kxo], name=kxo)
            for half in range(2):
                for kc in range(8):
                    S.op('pe', lambda e, half=half, kc=kc: e.matmul(P[half], lhsT=mixedT[:, kc, i * 128:(i + 1) * 128],
                                                                   rhs=wout[:, kc, half * 512:(half + 1) * 512], start=(kc == 0), stop=(kc == 7)),
                         reads=['d0p'], writes=[f'ps{half}'], signal=(kc == 7))
                S.op('dve', lambda e, half=half: e.tensor_tensor(out=acc[:, i, half * 512:(half + 1) * 512], in0=P[half],
                                                                 in1=xot[:, half * 512:(half + 1) * 512], op=ALU.add),
                     reads=[f'ps{half}', kxo], writes=[('acc', i)])
            S.op('dve', lambda e: e.tensor_tensor(out=xn, in0=acc[:, i, :], in1=acc[:, i, :], op=ALU.mult), reads=[('acc', i)], writes=['xn'])
            S.op('dve', lambda e: e.tensor_reduce(out=std[:, 0:1], in_=xn, axis=AX.X, op=ALU.add), reads=['xn'], writes=['ss2'])
            rsqrt_act(std[:, 0:1], std[:, 0:1], 1.0 / 1024.0, ['ss2', 'epsD'], ['ss2'])
            S.op('dve', lambda e: e.scalar_tensor_tensor(out=xn, in0=acc[:, i, :], scalar=std[:, 0:1], in1=g2bc, op0=ALU.mult, op1=ALU.mult),
                 reads=[('acc', i), 'ss2', 'd0'], writes=['xn'])
            if D_LEVEL < 2:
                return
            S.op('dve', lambda e: e.tensor_copy(out=xh, in_=xn), reads=['xn'], writes=['xh'])
            S.op('dve', lambda e: e.tensor_tensor(out=xl, in0=xn, in1=xh, op=ALU.subtract), reads=['xn', 'xh'], writes=['xl'])
            for c in range(8):
                S.op('pe', lambda e, c=c: e.transpose(PB[2][:, c * 128:(c + 1) * 128], xh[:, c * 128:(c + 1) * 128], identb2),
                     reads=['xh', 'identb2'], writes=['ps2'], signal=(c == 7))
            for c in range(8):
                S.op('pe', lambda e, c=c: e.transpose(PB[3][:, c * 128:(c + 1) * 128], xl[:, c * 128:(c + 1) * 128], identb2),
                     reads=['xl', 'identb2'], writes=['ps3'], signal=(c == 7))
            S.op('dve', lambda e: e.tensor_copy(out=xnT[:, :, i * 128:(i + 1) * 128], in_=PB[2].rearrange("p (c t) -> p c t", t=128)),
                 reads=['ps2'], writes=[('xnT', i)])
            S.op('dve', lambda e: e.tensor_copy(out=loT, in_=PB[3].rearrange("p (c t) -> p c t", t=128)), reads=['ps3'], writes=['loT'])
            if D_LEVEL < 3:
                return
            nmm = 0
            for (lt, kl, wt, kw_) in ((0, ('xnT', i), wrh, 'wrh'), (1, 'loT', wrh, 'wrh'), (0, ('xnT', i), wrl, 'wrl')):
                for kc in range(8):
                    lhs = xnT[:, kc, i * 128:(i + 1) * 128] if lt == 0 else loT[:, kc, :]
                    S.op('pe', lambda e, lhs=lhs, wt=wt, kc=kc, first=(nmm == 0), last=(nmm == 23): e.matmul(
                        P[4][:, 0:32], lhsT=lhs, rhs=wt[:, kc, :], start=first, stop=last),
                         reads=[kl, kw_], writes=['ps4'], signal=(nmm == 23))
                    nmm += 1
            S.op('dve', lambda e: e.tensor_tensor(out=lgt, in0=P[4][:, 0:32], in1=brow, op=ALU.add), reads=['ps4', 'd0'], writes=['lgt'])
            S.op('dve', lambda e: e.max(out=std[:, 8:16], in_=lgt), reads=['lgt'], writes=['m8r'])
            S.op('dve', lambda e: e.tensor_scalar(out=msk, in0=lgt, scalar1=std[:, 11:12], scalar2=None, op0=ALU.is_ge), reads=['lgt', 'm8r'], writes=['msk'])
            S.op('dve', lambda e: e.tensor_scalar(out=std[:, 16:17], in0=std[:, 8:9], scalar1=-1.0, scalar2=None, op0=ALU.mult), reads=['m8r'], writes=['negm'])
            S.op('act', lambda e: e.activation(out=ex, in_=lgt, func=AF.Exp, bias=std[:, 16:17]), reads=['lgt', 'negm'], writes=['ex'])
            S.op('dve', lambda e: e.tensor_tensor(out=ex, in0=ex, in1=msk, op=ALU.mult), reads=['ex', 'msk'], writes=['ex'])
            S.op('dve', lambda e: e.tensor_reduce(out=std[:, 17:18], in_=ex, axis=AX.X, op=ALU.add), reads=['ex'], writes=['se'])
            S.op('dve', lambda e: e.reciprocal(out=std[:, 17:18], in_=std[:, 17:18]), reads=['se'], writes=['se'])
            S.op('dve', lambda e: e.tensor_scalar(out=gmoe[:, i, :], in0=ex, scalar1=std[:, 17:18], scalar2=None, op0=ALU.mult),
                 reads=['ex', 'se'], writes=[('gmoe', i)])

        for i in range(NOWN):
            dpre(i)
        S.barrier()
        if debug:
            dbg("x1_0", acc[:, 0, :], 1024)
            dbg("gmoe", gmoe.rearrange("p a b -> p (a b)"), 512)
        if stop_after == 'Dpre':
            return _finish()

        A.top = dA
        t1 = [A.alloc([512], F32) for _ in range(2)]
        t2 = [A.alloc([512], F32) for _ in range(2)]
        t3 = [A.alloc([512], F32) for _ in range(2)]
        bdrow = [A.alloc([1024], BF16) for _ in range(2)]
        ones128 = A.alloc([128], BF16)
        assert A.top <= mixed_off
        S.op('dve', lambda e: e.memset(ones128, 1.0), writes=['ones128'])
        bgv = bgu.rearrange("p (a t) -> p a t", t=2)
        S.op('dve', lambda e: e.tensor_scalar(out=bgv[:, :, 1:2], in0=bgv[:, :, 1:2], scalar1=1.0, scalar2=None, op0=ALU.add),
             reads=['d0'], writes=['bgu1'])
        A.top = mixed_off
        aT = A.alloc([8, 2048], BF16)
        A.top = dB
        Wd = A.alloc([8, 1024], BF16)
        Wg = [A.alloc([8, 2, 128], BF16) for _ in range(4)]
        neg6 = A.alloc([512], F32)
        S.op('dve', lambda e: e.memset(neg6, -6.0), writes=['neg6'])

        cnt = [0, 0, 0]

        chunks = [(e_, m_) for e_ in range(n_exp) for m_ in range(8)]

        def issue(n):
            e_, m_ = chunks[n]
            wg, kwg = Wg[n % 4], f'Wg{n % 4}'
            S.dma('pool', wg.rearrange("p a b c -> p (a b c)"), wgu_d[e_ * 8 + m_], writes=[kwg], name=kwg)

        for n0 in range(min(3, len(chunks))):
            issue(n0)

        def expert(ex_):
            S.dma('pool', Wd.rearrange("p a b -> p (a b)"), wd_d[ex_], writes=['Wd'], name='Wd')
            bdr, kbd = bdrow[ex_ % 2], f'bdrow{ex_ % 2}'
            S.dma('pool', bdr[0:1, :], b_down_d[ex_:ex_ + 1, :], writes=[kbd], name=kbd)
            for m in range(8):
                wn = cnt[0]
                cnt[0] += 1
                if wn + 3 < len(chunks):
                    issue(wn + 3)
                wg, kwg = Wg[wn % 4], f'Wg{wn % 4}'
                for tg in range(4):
                    n = cnt[1]
                    cnt[1] += 1
                    pg, pl = P[(2 * n) % 4], P[(2 * n + 1) % 4]
                    kpg, kpl = f'ps{(2 * n) % 4}', f'ps{(2 * n + 1) % 4}'
                    a1, a2, a3 = t1[n % 2], t2[n % 2], t3[n % 2]
                    k1, k2, k3 = f't1{n % 2}', f't2{n % 2}', f't3{n % 2}'
                    for gl, pp, kp in ((0, pg, kpg), (1, pl, kpl)):
                        for kc in range(8):
                            S.op('pe', lambda e, gl=gl, pp=pp, kc=kc, wg=wg, tg=tg: e.matmul(
                                pp, lhsT=wg[:, kc, gl, :], rhs=xnT[:, kc, tg * 512:(tg + 1) * 512], start=(kc == 0), stop=(kc == 7)),
                                 reads=[kwg], writes=[kp], signal=(kc == 7))
                    bcol = ex_ * 16 + m * 2
                    S.op('dve', lambda e, pg=pg, a1=a1, bcol=bcol: e.tensor_scalar(out=a1, in0=pg, scalar1=bgu[:, bcol:bcol + 1], scalar2=7.0,
                                                                                   op0=ALU.add, op1=ALU.min), reads=[kpg, 'bgu1'], writes=[k1])
                    S.op('act', lambda e, a1=a1, a2=a2: e.activation(out=a2, in_=a1, func=AF.Sigmoid, scale=1.702), reads=[k1], writes=[k2])
                    S.op('dve', lambda e, pl=pl, a3=a3, bcol=bcol: e.tensor_scalar(out=a3, in0=pl, scalar1=bgu[:, bcol + 1:bcol + 2], scalar2=8.0,
                                                                                   op0=ALU.add, op1=ALU.min), reads=[kpl, 'bgu1'], writes=[k3])
                    S.op('pool', lambda e, a1=a1, a2=a2: e.tensor_tensor(out=a2, in0=a1, in1=a2, op=ALU.mult), reads=[k1, k2], writes=[k2])
                    S.op('dve', lambda e, a3=a3: e.tensor_scalar(out=a3, in0=a3, scalar1=-6.0, scalar2=None, op0=ALU.max), reads=[k3], writes=[k3])
                    S.op('pool', lambda e, a2=a2, a3=a3, m=m, tg=tg: e.tensor_tensor(
                        out=aT[:, m, tg * 512:(tg + 1) * 512], in0=a3, in1=a2, op=ALU.mult),
                         reads=[k2, k3], writes=['aT'])
            for tt in range(16):
                for half in range(2):
                    k = cnt[2]
                    cnt[2] += 1
                    py, kpy = P[4 + k % 4], f'ps{4 + k % 4}'
                    for m in range(8):
                        S.op('pe', lambda e, py=py, m=m, tt=tt, half=half: e.matmul(
                            py, lhsT=aT[:, m, tt * 128:(tt + 1) * 128], rhs=Wd[:, m, half * 512:(half + 1) * 512], start=(m == 0), stop=False),
                             reads=['aT', 'Wd'], writes=[kpy], signal=False)
                    S.op('pe', lambda e, py=py, half=half: e.matmul(py, lhsT=ones128[0:1, :], rhs=bdr[0:1, half * 512:(half + 1) * 512],
                                                                   start=False, stop=True), reads=[kbd, 'ones128'], writes=[kpy])
                    S.op('dve', lambda e, py=py, tt=tt, half=half: e.scalar_tensor_tensor(
                        out=acc[:, tt, half * 512:(half + 1) * 512], in0=py, scalar=gmoe[:, tt, ex_:ex_ + 1],
                        in1=acc[:, tt, half * 512:(half + 1) * 512], op0=ALU.mult, op1=ALU.add),
                         reads=[kpy, ('acc', tt)], writes=[('acc', tt)])

        for ex_ in range(n_exp):
            expert(ex_)
        for i in range(NOWN):
            S.dma('sp', out_d[i], acc[:, i, :], reads=[('acc', i)], name='out')
        return _finish()


_CACHE = {}


def kernel(**inputs):
    inputs = {k: np.asarray(v) for k, v in inputs.items()}
    x = inputs["x"].astype(np.float32)
    W = _host_weights(inputs)
    if "nc" not in _CACHE:
        _CACHE["nc"] = build_program()
    nc, dbg_outs = _CACHE["nc"]
    in_maps = []
    for c in range(8):
        b, j = c // 4, c % 4
        pad = 3 - j
        xb = x[b]
        xp = np.concatenate([np.zeros((pad * 128, D), np.float32), xb], axis=0)[:8192]
        xT = np.ascontiguousarray(xp.reshape(64, 128, 8, 128).transpose(0, 3, 2, 1)).reshape(64, 128, 1024)
        xown = np.ascontiguousarray(xb.reshape(16, 4, 128, D)[:, j])
        m = dict(xT=xT, xown=xown)
        m.update(_host_consts(j))
        m.update(W)
        in_maps.append(m)
    res = run_bass_kernel_spmd(nc, in_maps, core_ids=list(range(8)))
    out = np.zeros((2, 8192, D), np.float32)
    ov = out.reshape(2, 16, 4, 128, D)
    for c in range(8):
        b, j = c // 4, c % 4
        ov[b, :, j] = res.results[c]["out"]
    _CACHE["last"] = res
    return out
```
